# Optimizing a Trainium2 kernel written in Bass

```python
import numpy as np
import jax
import jax.numpy as jnp
from jax import lax

D_MODEL = 1024
BATCH = 8
SEQ = 2048
DEPTH = 1

HEAD_DIM = 64
NSA_HEADS = 8
NSA_KV_HEADS = 2
NSA_GROUP = NSA_HEADS // NSA_KV_HEADS
SWA_HEADS = 8
SWA_KV_HEADS = 2
SWA_GROUP = SWA_HEADS // SWA_KV_HEADS
NSA_WIDTH = NSA_HEADS * HEAD_DIM
SWA_WIDTH = SWA_HEADS * HEAD_DIM
NSA_KV_WIDTH = NSA_KV_HEADS * HEAD_DIM
SWA_KV_WIDTH = SWA_KV_HEADS * HEAD_DIM
CMP_LEN = 32
CMP_STRIDE = 16
CMP_HIDDEN = 256
SLC_LEN = 64
SLC_TOPK = 8
NSA_WINDOW = 256
SLC_QUERY_CHUNK = 64
SWA_WINDOW = 128
BAND_BLOCK = 128
N_GROUPS = 4
EXPERTS_PER_GROUP = 8
N_EXPERTS = N_GROUPS * EXPERTS_PER_GROUP
EXPERT_TOPK = 2
EXPERT_FF = 256
MOE_BLOCK = 128
IN_SIZES = (NSA_WIDTH, NSA_KV_WIDTH, NSA_KV_WIDTH, NSA_KV_WIDTH, NSA_KV_WIDTH, NSA_KV_WIDTH, NSA_KV_WIDTH,
            3 * NSA_HEADS, SWA_WIDTH, SWA_KV_WIDTH, SWA_KV_WIDTH, 2 * D_MODEL)
IN_WIDTH = sum(IN_SIZES)
RMS_EPS = 1e-6
NEG_INF = -1e30
FORCE_SCORE = 1e9

kernel_name = "hybrid_nsa_swa_sinks_hier_moe"


def rms_norm(x, g):
    x32 = x.astype(jnp.float32)
    y = x32 * lax.rsqrt(jnp.mean(x32 * x32, axis=-1, keepdims=True) + RMS_EPS)
    return (y * g.astype(jnp.float32)).astype(x.dtype)


def masked_softmax(s, mask):
    s = jnp.where(mask, s, NEG_INF)
    m = jnp.max(s, axis=-1, keepdims=True)
    e = jnp.where(mask, jnp.exp(s - m), 0.0)
    z = jnp.sum(e, axis=-1, keepdims=True)
    return e / jnp.where(z > 0, z, 1.0)


def alibi_slopes():
    n = NSA_HEADS + SWA_HEADS
    s = 2.0 ** (-8.0 * np.arange(1, n + 1) / n)
    return jnp.asarray(s[:SWA_HEADS], jnp.float32), jnp.asarray(s[SWA_HEADS:], jnp.float32)


def compress_blocks(kv, pos, w1, w2):
    B, S, H, d = kv.shape
    r = CMP_LEN // CMP_STRIDE
    nc = S // CMP_STRIDE - r + 1
    kc = kv.reshape(B, S // CMP_STRIDE, CMP_STRIDE, H, d)
    blocks = jnp.concatenate([kc[:, i:i + nc] for i in range(r)], axis=2)
    blocks = blocks + pos[None, None, :, None, :]
    flat = jnp.transpose(blocks, (0, 1, 3, 2, 4)).reshape(B, nc, H, CMP_LEN * d)
    return jax.nn.silu(flat @ w1) @ w2


def compressed_branch(q, kc, vc, slopes):
    B, S, Hkv, G, d = q.shape
    nc = kc.shape[1]
    s = jnp.einsum('bshgd,bchd->bhgsc', q, kc, preferred_element_type=jnp.float32) * (d ** -0.5)
    end = jnp.arange(nc) * CMP_STRIDE + CMP_LEN - 1
    dist = jnp.arange(S)[:, None] - end[None, :]
    s = s - slopes.reshape(Hkv, G, 1, 1) * dist.astype(jnp.float32)
    p = masked_softmax(s, dist >= 0)
    o = jnp.einsum('bhgsc,bchd->bshgd', p.astype(vc.dtype), vc)
    return o, p


def select_blocks(p_cmp, S):
    nc = p_cmp.shape[-1]
    ns = S // SLC_LEN
    c0 = np.arange(nc) * CMP_STRIDE
    s0 = np.arange(ns) * SLC_LEN
    ov = np.clip(np.minimum(c0[:, None] + CMP_LEN, s0[None, :] + SLC_LEN)
                 - np.maximum(c0[:, None], s0[None, :]), 0, None) / CMP_LEN
    imp = jnp.einsum('bhsc,cj->bhsj', jnp.sum(p_cmp, axis=2), jnp.asarray(ov, jnp.float32))
    t = jnp.arange(S)[:, None]
    j = jnp.arange(ns)[None, :]
    cur = t // SLC_LEN
    valid = j * SLC_LEN <= t
    forced = (j == 0) | (j == cur) | (j == cur - 1)
    score = jnp.where(forced, FORCE_SCORE, jnp.where(valid, imp, NEG_INF))
    vals, idx = lax.top_k(score, min(SLC_TOPK, ns))
    return idx, vals > 0.5 * NEG_INF


def selected_branch(q, k, v, idx, valid, slopes):
    B, S, Hkv, G, d = q.shape
    ns = S // SLC_LEN
    n = idx.shape[-1]
    QC = SLC_QUERY_CHUNK
    nq = S // QC
    kb = jnp.transpose(k.reshape(B, ns, SLC_LEN, Hkv, d), (0, 3, 1, 2, 4))
    vb = jnp.transpose(v.reshape(B, ns, SLC_LEN, Hkv, d), (0, 3, 1, 2, 4))
    qc = jnp.swapaxes(q.reshape(B, nq, QC, Hkv, G, d), 0, 1)
    ic = jnp.transpose(idx.reshape(B, Hkv, nq, QC, n), (2, 0, 1, 3, 4))
    vc = jnp.transpose(valid.reshape(B, Hkv, nq, QC, n), (2, 0, 1, 3, 4))
    tc = jnp.arange(S).reshape(nq, QC)
    bi = jnp.arange(B)[:, None, None, None]
    hi = jnp.arange(Hkv)[None, :, None, None]
    sl = slopes.reshape(1, Hkv, G, 1, 1, 1)
    scale = d ** -0.5

    def chunk(args):
        qq, ii, vv, tt = args
        kg = kb[bi, hi, ii]
        vg = vb[bi, hi, ii]
        s = jnp.einsum('bqhgd,bhqnld->bhgqnl', qq, kg, preferred_element_type=jnp.float32) * scale
        kpos = ii[..., None] * SLC_LEN + jnp.arange(SLC_LEN)
        dist = tt[None, None, :, None, None] - kpos
        mask = (vv[..., None] & (dist >= 0))[:, :, None]
        s = s - sl * dist[:, :, None].astype(jnp.float32)
        p = masked_softmax(s.reshape(B, Hkv, G, QC, n * SLC_LEN), mask.reshape(B, Hkv, 1, QC, n * SLC_LEN))
        return jnp.einsum('bhgqk,bhqkd->bqhgd', p.astype(vg.dtype), vg.reshape(B, Hkv, QC, n * SLC_LEN, d))

    out = lax.map(chunk, (qc, ic, vc, tc))
    return jnp.swapaxes(out, 0, 1).reshape(B, S, Hkv, G, d)


def banded_attention(q, k, v, slopes, window, sinks):
    B, S, Hkv, G, d = q.shape
    nb = S // BAND_BLOCK
    nprev = -(-(window - 1) // BAND_BLOCK)
    pad = nprev * BAND_BLOCK
    kw = (nprev + 1) * BAND_BLOCK

    def band(a):
        ap = jnp.pad(a, ((0, 0), (pad, 0), (0, 0), (0, 0))).reshape(B, nb + nprev, BAND_BLOCK, Hkv, d)
        return jnp.concatenate([ap[:, j:j + nb] for j in range(nprev + 1)], axis=2)

    kb, vb = band(k), band(v)
    qb = q.reshape(B, nb, BAND_BLOCK, Hkv, G, d)
    s = jnp.einsum('bnqhgd,bnkhd->bnhgqk', qb, kb, preferred_element_type=jnp.float32) * (d ** -0.5)
    blk = jnp.arange(nb)[:, None] * BAND_BLOCK
    tpos = blk + jnp.arange(BAND_BLOCK)
    kpos = blk - pad + jnp.arange(kw)
    dist = tpos[:, :, None] - kpos[:, None, :]
    mask = ((dist >= 0) & (dist < window) & (kpos[:, None, :] >= 0))[None, :, None, None]
    s = s - slopes.reshape(1, 1, Hkv, G, 1, 1) * dist.astype(jnp.float32)[None, :, None, None]
    s = jnp.where(mask, s, NEG_INF)
    m = jnp.max(s, axis=-1, keepdims=True)
    if sinks is not None:
        sk = sinks.astype(jnp.float32).reshape(1, 1, Hkv, G, 1, 1)
        m = jnp.maximum(m, sk)
    e = jnp.where(mask, jnp.exp(s - m), 0.0)
    z = jnp.sum(e, axis=-1, keepdims=True)
    if sinks is not None:
        z = z + jnp.exp(sk - m)
    p = e / z
    o = jnp.einsum('bnhgqk,bnkhd->bnqhgd', p.astype(v.dtype), vb)
    return o.reshape(B, S, Hkv, G, d)


def hier_moe(h, w_group, b_group, w_expert, b_expert, w_gate_e, w_up_e, w_down_e):
    B, S, D = h.shape
    N = B * S
    hf = h.reshape(N, D)
    g_logits = (hf @ w_group).astype(jnp.float32) + b_group.astype(jnp.float32)
    g_prob = jax.nn.softmax(g_logits, axis=-1)
    grp = jnp.argmax(g_logits, axis=-1)
    p_grp = jnp.take_along_axis(g_prob, grp[:, None], axis=-1)[:, 0]
    e_logits = ((hf @ w_expert).astype(jnp.float32) + b_expert.astype(jnp.float32)).reshape(N, N_GROUPS, EXPERTS_PER_GROUP)
    e_in = jnp.take_along_axis(e_logits, grp[:, None, None], axis=1)[:, 0]
    top_v, top_i = lax.top_k(e_in, EXPERT_TOPK)
    weight = p_grp[:, None] * jax.nn.softmax(top_v, axis=-1)
    eid = (grp[:, None] * EXPERTS_PER_GROUP + top_i).reshape(-1)
    w_f = weight.reshape(-1)
    NK = N * EXPERT_TOPK
    tok = jnp.arange(NK) // EXPERT_TOPK
    order = jnp.argsort(eid)
    e_sorted = eid[order]
    counts = jnp.bincount(eid, length=N_EXPERTS)
    padded = (counts + MOE_BLOCK - 1) // MOE_BLOCK * MOE_BLOCK
    pad_end = jnp.cumsum(padded)
    pad_start = pad_end - padded
    cnt_start = jnp.cumsum(counts) - counts
    dest = pad_start[e_sorted] + jnp.arange(NK) - cnt_start[e_sorted]
    n_blocks = -(-(NK + N_EXPERTS * (MOE_BLOCK - 1)) // MOE_BLOCK)
    buf = jnp.zeros((n_blocks * MOE_BLOCK, D), h.dtype).at[dest].set(hf[tok[order]])
    block_e = jnp.minimum(jnp.searchsorted(pad_end, jnp.arange(n_blocks) * MOE_BLOCK, side='right'), N_EXPERTS - 1)

    def expert_block(args):
        xb, e = args
        return (jax.nn.silu(xb @ w_gate_e[e]) * (xb @ w_up_e[e])) @ w_down_e[e]

    yb = lax.map(expert_block, (buf.reshape(n_blocks, MOE_BLOCK, D), block_e)).reshape(-1, D)
    y_slot = yb[dest] * w_f[order][:, None].astype(yb.dtype)
    y = jnp.zeros((N, D), h.dtype).at[tok[order]].add(y_slot)
    return y.reshape(B, S, D)


def setup_inputs(seed: int = 0) -> dict:
    key = jax.random.key(seed)
    ks = jax.random.split(key, 24)
    f32 = jnp.float32
    L = DEPTH
    d = HEAD_DIM

    def nrm(k, shape, scale):
        return jax.random.normal(k, shape, f32) * scale

    return {
        "x": nrm(ks[0], (BATCH, SEQ, D_MODEL), 1.0),
        "norm_mix_g": 1.0 + nrm(ks[1], (L, D_MODEL), 0.02),
        "w_in": nrm(ks[2], (L, D_MODEL, IN_WIDTH), D_MODEL ** -0.5),
        "cmp_pos_k": nrm(ks[3], (L, CMP_LEN, d), 0.1),
        "cmp_w1_k": nrm(ks[4], (L, CMP_LEN * d, CMP_HIDDEN), (CMP_LEN * d) ** -0.5),
        "cmp_w2_k": nrm(ks[5], (L, CMP_HIDDEN, d), CMP_HIDDEN ** -0.5),
        "cmp_pos_v": nrm(ks[6], (L, CMP_LEN, d), 0.1),
        "cmp_w1_v": nrm(ks[7], (L, CMP_LEN * d, CMP_HIDDEN), (CMP_LEN * d) ** -0.5),
        "cmp_w2_v": nrm(ks[8], (L, CMP_HIDDEN, d), CMP_HIDDEN ** -0.5),
        "sinks": nrm(ks[9], (L, SWA_HEADS), 0.5),
        "w_a": nrm(ks[10], (L, NSA_WIDTH, D_MODEL), NSA_WIDTH ** -0.5),
        "w_b": nrm(ks[11], (L, SWA_WIDTH, D_MODEL), SWA_WIDTH ** -0.5),
        "w_o": nrm(ks[12], (L, D_MODEL, D_MODEL), D_MODEL ** -0.5),
        "norm_ffn_g": 1.0 + nrm(ks[13], (L, D_MODEL), 0.02),
        "w_group": nrm(ks[14], (L, D_MODEL, N_GROUPS), D_MODEL ** -0.5),
        "b_group": nrm(ks[15], (L, N_GROUPS), 0.01),
        "w_expert": nrm(ks[16], (L, D_MODEL, N_EXPERTS), D_MODEL ** -0.5),
        "b_expert": nrm(ks[17], (L, N_EXPERTS), 0.01),
        "w_gate_e": nrm(ks[18], (L, N_EXPERTS, D_MODEL, EXPERT_FF), D_MODEL ** -0.5),
        "w_up_e": nrm(ks[19], (L, N_EXPERTS, D_MODEL, EXPERT_FF), D_MODEL ** -0.5),
        "w_down_e": nrm(ks[20], (L, N_EXPERTS, EXPERT_FF, D_MODEL), EXPERT_FF ** -0.5),
        "norm_final_g": 1.0 + nrm(ks[21], (D_MODEL,), 0.02),
    }


def reference(x, norm_mix_g, w_in, cmp_pos_k, cmp_w1_k, cmp_w2_k, cmp_pos_v, cmp_w1_v, cmp_w2_v,
              sinks, w_a, w_b, w_o, norm_ffn_g, w_group, b_group, w_expert, b_expert,
              w_gate_e, w_up_e, w_down_e, norm_final_g):
    B, S, _ = x.shape
    slopes_swa, slopes_nsa = alibi_slopes()
    split_points = [int(c) for c in np.cumsum(IN_SIZES)[:-1]]
    for l in range(DEPTH):
        h = rms_norm(x, norm_mix_g[l])
        proj = h @ w_in[l]
        (q_a, k_cmp, v_cmp, k_slc, v_slc, k_win, v_win, g_nsa,
         q_b, k_b, v_b, g_merge) = jnp.split(proj, split_points, axis=-1)
        q_a = q_a.reshape(B, S, NSA_KV_HEADS, NSA_GROUP, HEAD_DIM)
        kv_a = [t.reshape(B, S, NSA_KV_HEADS, HEAD_DIM) for t in (k_cmp, v_cmp, k_slc, v_slc, k_win, v_win)]
        kc = compress_blocks(kv_a[0], cmp_pos_k[l], cmp_w1_k[l], cmp_w2_k[l])
        vc = compress_blocks(kv_a[1], cmp_pos_v[l], cmp_w1_v[l], cmp_w2_v[l])
        o_cmp, p_cmp = compressed_branch(q_a, kc, vc, slopes_nsa)
        idx, valid = select_blocks(p_cmp, S)
        o_slc = selected_branch(q_a, kv_a[2], kv_a[3], idx, valid, slopes_nsa)
        o_win = banded_attention(q_a, kv_a[4], kv_a[5], slopes_nsa, NSA_WINDOW, None)
        g = jax.nn.sigmoid(g_nsa).reshape(B, S, NSA_KV_HEADS, NSA_GROUP, 3)
        o_a = (g[..., 0:1] * o_cmp + g[..., 1:2] * o_slc + g[..., 2:3] * o_win).reshape(B, S, NSA_WIDTH)
        q_b = q_b.reshape(B, S, SWA_KV_HEADS, SWA_GROUP, HEAD_DIM)
        k_b = k_b.reshape(B, S, SWA_KV_HEADS, HEAD_DIM)
        v_b = v_b.reshape(B, S, SWA_KV_HEADS, HEAD_DIM)
        o_b = banded_attention(q_b, k_b, v_b, slopes_swa, SWA_WINDOW, sinks[l]).reshape(B, S, SWA_WIDTH)
        gate_a, gate_b = jnp.split(jax.nn.sigmoid(g_merge), 2, axis=-1)
        mix = (gate_a * (o_a @ w_a[l]) + gate_b * (o_b @ w_b[l])) @ w_o[l]
        x = x + mix
        x = x + hier_moe(rms_norm(x, norm_ffn_g[l]), w_group[l], b_group[l], w_expert[l], b_expert[l],
                         w_gate_e[l], w_up_e[l], w_down_e[l])
    return rms_norm(x, norm_final_g)
```

```python
import contextlib
import ml_dtypes
from concourse.bass_utils import run_bass_kernel_spmd
import numpy as np
import concourse.bass as bass
import concourse.mybir as mybir

F32 = mybir.dt.float32
BF16 = mybir.dt.bfloat16
I32 = mybir.dt.int32
ALU = mybir.AluOpType
AF = mybir.ActivationFunctionType
AX = mybir.AxisListType

_DT_SIZE = {F32: 4, BF16: 2, I32: 4}


class View:
    __slots__ = ("buf", "ap", "box", "boxes")

    def __init__(self, buf, ap, box, boxes=None):
        self.buf = buf
        self.ap = ap
        self.box = box
        self.boxes = boxes if boxes is not None else [box]

    def with_ap(self, fn):
        return View(self.buf, fn(self.ap), self.box, self.boxes)


class Buf:
    def __init__(self, handle, shape, dtype, name, base_byte=0, alias=None):
        self.h = handle
        self.shape = tuple(shape)
        self.dtype = dtype
        self.name = name
        self.esz = _DT_SIZE[dtype]
        self.base_byte = base_byte
        self.alias = alias if alias is not None else self
        self.psum = False
        st = []
        s = 1
        for d in reversed(self.shape[1:]):
            st.append(s)
            s *= d
        self.fstrides = list(reversed(st))
        self.wr = []
        self.rd = {}

    def __getitem__(self, key):
        if not isinstance(key, tuple):
            key = (key,)
        key = list(key) + [slice(None)] * (len(self.shape) - len(key))
        k0 = key[0]
        if isinstance(k0, slice):
            p0, p1, _ = k0.indices(self.shape[0])
        else:
            p0, p1 = k0, k0 + 1
            key[0] = slice(k0, k0 + 1)
        lo = 0
        hi = 0
        for d, k in enumerate(key[1:]):
            n = self.shape[d + 1]
            if isinstance(k, slice):
                a, b, step = k.indices(n)
                cnt = len(range(a, b, step))
                last = a + (cnt - 1) * step
            else:
                a = k
                last = k
            lo += a * self.fstrides[d]
            hi += last * self.fstrides[d]
        box = (p0, p1, self.base_byte + lo * self.esz, self.base_byte + (hi + 1) * self.esz)
        if self.alias.psum:
            box = (0, 128, 0, 2048)
        return View(self.alias, self.h[tuple(key)], box)


def _overlap(a, b):
    return a[0] < b[1] and b[0] < a[1] and a[2] < b[3] and b[2] < a[3]


def _contains(outer, inner):
    return outer[0] <= inner[0] and outer[1] >= inner[1] and outer[2] <= inner[2] and outer[3] >= inner[3]


class Sched:
    ENGS = ("sync", "act", "dve", "pool", "pe")
    NSLOT = 6

    def __init__(self, nc):
        self.nc = nc
        self.ops = []
        self.eng_ops = {e: [] for e in self.ENGS}
        self.dma_count = {e: 0 for e in self.ENGS}
        self.dma_ops = {e: [] for e in self.ENGS}
        self.out_dmas = []
        self.debug = False

    MAXREC = 12

    def _is_inorder(self, oid):
        o = self.ops[oid]
        return not o["dma"]

    def op(self, eng, fn, reads=(), writes=(), dma=False, is_out=False):
        oid = len(self.ops)
        deps = set()
        reads = [(v.buf, bx) for v in reads for bx in v.boxes]
        writes = [(v.buf, bx) for v in writes for bx in v.boxes]
        for (vb, vbox) in reads:
            for (box, o) in vb.wr:
                if _overlap(box, vbox):
                    deps.add(o)
        for (vb, vbox) in writes:
            for (box, o) in vb.wr:
                if _overlap(box, vbox):
                    deps.add(o)
            for key, lst in vb.rd.items():
                if key == eng and not dma and eng == "pe":
                    continue
                for (box, o) in lst:
                    if _overlap(box, vbox):
                        deps.add(o)
        rec = dict(eng=eng, fn=fn, deps=deps, dma=dma, id=oid)
        if self.debug:
            import sys as _sys
            f = _sys._getframe(1)
            w = []
            while f is not None and len(w) < 4:
                if f.f_code.co_name not in ("op", "dma", "mm", "act", "tt", "ts", "stt", "copy", "memset", "recip", "reduce", "max8", "transpose"):
                    w.append("%s:%d" % (f.f_code.co_name, f.f_lineno))
                f = f.f_back
            rec["where"] = " < ".join(w)
        self.ops.append(rec)
        for (b, vbox) in writes:
            b.wr = [r for r in b.wr if not _contains(vbox, r[0])]
            for key in list(b.rd.keys()):
                b.rd[key] = [r for r in b.rd[key] if not _contains(vbox, r[0])]
            b.wr.append((vbox, oid))
            if len(b.wr) > 4 * self.MAXREC:
                self._merge_writes(b)
        for (b, vbox) in reads:
            key = ("dma", oid) if dma else eng
            lst = b.rd.setdefault(key, [])
            lst[:] = [r for r in lst if r[0] != vbox]
            lst.append((vbox, oid))
            if len(lst) > self.MAXREC:
                p0 = min(r[0][0] for r in lst)
                p1 = max(r[0][1] for r in lst)
                b0 = min(r[0][2] for r in lst)
                b1 = max(r[0][3] for r in lst)
                lst[:] = [((p0, p1, b0, b1), oid)]
        if dma:
            i = self.dma_count[eng]
            self.dma_count[eng] += 1
            rec["dma_i"] = i
            if i >= self.NSLOT:
                deps.add(self.dma_ops[eng][i - self.NSLOT])
            self.dma_ops[eng].append(oid)
            if is_out:
                self.out_dmas.append(oid)
        rec["seq"] = len(self.eng_ops[eng])
        self.eng_ops[eng].append(oid)
        return oid

    def _merge_writes(self, b):
        groups = {}
        keep = []
        for (box, o) in b.wr:
            op_ = self.ops[o]
            if op_["dma"]:
                keep.append((box, o))
            else:
                groups.setdefault(op_["eng"], []).append((box, o))
        for e, lst in groups.items():
            if len(lst) <= 4:
                keep.extend(lst)
                continue
            p0 = min(r[0][0] for r in lst)
            p1 = max(r[0][1] for r in lst)
            b0 = min(r[0][2] for r in lst)
            b1 = max(r[0][3] for r in lst)
            keep.append(((p0, p1, b0, b1), max(r[1] for r in lst)))
        b.wr = keep

    def emit(self):
        nc = self.nc
        ops = self.ops
        fin = dict(eng="sync", fn=None, deps=set(self.out_dmas), dma=False, id=len(ops), seq=len(self.eng_ops["sync"]))
        self.eng_ops["sync"].append(fin["id"])
        ops.append(fin)
        raw_same = {}
        needed = [False] * len(ops)
        for o in ops:
            for d in o["deps"]:
                dop = ops[d]
                if dop["dma"]:
                    continue
                if dop["eng"] == o["eng"] and not o["dma"]:
                    if o["eng"] == "pe" or o["eng"] == "sync":
                        continue
                needed[d] = True
        cv = {}
        for e in self.ENGS:
            c = 0
            for oid in self.eng_ops[e]:
                o = ops[oid]
                if o["dma"] or o["fn"] is None:
                    continue
                if needed[oid]:
                    c += 1
                    cv[oid] = c
        import contextlib
        with contextlib.ExitStack() as st:
            sem_eng = {e: st.enter_context(nc.semaphore("s_" + e)) for e in ("act", "dve", "pool", "pe")}
            sem_dma = {e: [st.enter_context(nc.semaphore("d_%s%d" % (e, i))) for i in range(self.NSLOT)]
                       for e in ("sync", "act", "pool") if self.dma_count[e] > 0}
            block = st.enter_context(nc.Block())
            sched = self

            def run_engine(ename, e):
                seen = {x: 0 for x in ("act", "dve", "pool", "pe")}
                seen_dma = {}
                for oid in sched.eng_ops[ename]:
                    o = ops[oid]
                    for d in sorted(o["deps"]):
                        dop = ops[d]
                        if dop["dma"]:
                            q = dop["eng"]
                            i = dop["dma_i"]
                            slot = i % sched.NSLOT
                            val = 16 * (i // sched.NSLOT + 1)
                            if seen_dma.get((q, slot), 0) >= val:
                                continue
                            e.wait_ge(sem_dma[q][slot], val)
                            seen_dma[(q, slot)] = val
                        else:
                            de = dop["eng"]
                            if de == ename and not o["dma"] and ename in ("pe", "sync"):
                                continue
                            if d not in cv:
                                continue
                            val = cv[d]
                            if seen[de] >= val:
                                continue
                            e.wait_ge(sem_eng[de], val)
                            seen[de] = val
                    if o["fn"] is None:
                        continue
                    ins = o["fn"](e)
                    if sched.debug:
                        ins.annotate(o["where"])
                    if o["dma"]:
                        i = o["dma_i"]
                        ins.then_inc(sem_dma[ename][i % sched.NSLOT], 16)
                    elif needed[oid]:
                        ins.then_inc(sem_eng[ename], 1)

            @block.sync
            def _(e):
                run_engine("sync", e)

            @block.scalar
            def _(e):
                run_engine("act", e)

            @block.vector
            def _(e):
                run_engine("dve", e)

            @block.gpsimd
            def _(e):
                run_engine("pool", e)

            @block.tensor
            def _(e):
                run_engine("pe", e)

    def dma(self, out, in_, eng="sync", is_out=False, **kw):
        reads = [in_] if isinstance(in_, View) else []
        writes = [out] if isinstance(out, View) else []
        oa = out.ap if isinstance(out, View) else out
        ia = in_.ap if isinstance(in_, View) else in_
        return self.op(eng, lambda e: e.dma_start(out=oa, in_=ia, **kw), reads, writes, dma=True, is_out=is_out)

    def mm(self, out, lhsT, rhs, start=True, stop=True, **kw):
        return self.op("pe", lambda e: e.matmul(out.ap, lhsT.ap, rhs.ap, start=start, stop=stop, **kw),
                       [lhsT, rhs], [out])

    def transpose(self, out, in_, ident):
        return self.op("pe", lambda e: e.transpose(out.ap, in_.ap, ident.ap), [in_, ident], [out])

    def act(self, out, in_, func, bias=None, scale=None, accum=None, eng="act"):
        reads = [in_]
        kw = {}
        if bias is not None:
            if isinstance(bias, View):
                reads.append(bias)
                kw["bias"] = bias.ap
            else:
                kw["bias"] = bias
        if scale is not None:
            if isinstance(scale, View):
                reads.append(scale)
                kw["scale"] = scale.ap
            else:
                kw["scale"] = scale
        writes = [out]
        if accum is not None:
            writes.append(accum)
            kw["accum_out"] = accum.ap
        return self.op(eng, lambda e: e.activation(out.ap, in_.ap, func, **kw), reads, writes)

    def tt(self, out, in0, in1, op, eng="dve"):
        return self.op(eng, lambda e: e.tensor_tensor(out.ap, in0.ap, in1.ap, op), [in0, in1], [out])

    def ts(self, out, in0, s1, s2, op0, op1=None, eng="dve", accum=None):
        reads = [in0]
        a1 = s1
        a2 = s2
        if isinstance(s1, View):
            reads.append(s1)
            a1 = s1.ap
        if isinstance(s2, View):
            reads.append(s2)
            a2 = s2.ap
        writes = [out]
        kw = {}
        if op1 is not None:
            kw["op1"] = op1
        if accum is not None:
            writes.append(accum)
            kw["accum_out"] = accum.ap
        return self.op(eng, lambda e: e.tensor_scalar(out.ap, in0.ap, a1, a2, op0, **kw), reads, writes)

    def stt(self, out, in0, scalar, in1, op0, op1, eng="dve"):
        reads = [in0, in1]
        sc = scalar
        if isinstance(scalar, View):
            reads.append(scalar)
            sc = scalar.ap
        return self.op(eng, lambda e: e.scalar_tensor_tensor(out.ap, in0.ap, sc, in1.ap, op0, op1), reads, [out])

    def copy(self, out, in_, eng="dve"):
        if eng == "act":
            return self.op(eng, lambda e: e.copy(out.ap, in_.ap), [in_], [out])
        return self.op(eng, lambda e: e.tensor_copy(out.ap, in_.ap), [in_], [out])

    def memset(self, out, val, eng="pool"):
        return self.op(eng, lambda e: e.memset(out.ap, val), [], [out])

    def recip(self, out, in_):
        return self.op("dve", lambda e: e.reciprocal(out.ap, in_.ap), [in_], [out])

    def reduce(self, out, in_, op, axis=AX.X, eng="dve"):
        return self.op(eng, lambda e: e.tensor_reduce(out.ap, in_.ap, axis, op), [in_], [out])

    def max8(self, out, in_):
        return self.op("dve", lambda e: e.max(out.ap, in_.ap), [in_], [out])


SEQ = 2048
DM = 1024
NT = 16
NEG = -30000.0
BF = ml_dtypes.bfloat16


def _slopes():
    n = 16
    s = 2.0 ** (-8.0 * np.arange(1, n + 1) / n)
    return s[:8].astype(np.float64), s[8:].astype(np.float64)


CB = {}
CF = {}


def _layout():
    o = 0
    for name, n in (("identb", 128), ("esel", SEQ), ("cmpmask", SEQ), ("ovp", 33), ("tri", 128),
                    ("band_win", 8 * 3 * 128), ("band_swa", 8 * 2 * 128)):
        CB[name] = (o, n)
        o += n
    CB["_n"] = o + (o % 2)
    o = 0
    for name, n in (("identf", 128), ("forced", NT * 32), ("bias_cmp", 32), ("bias_slc", 8 * 19),
                    ("bias_band", 16), ("sq_swa", 8)):
        CF[name] = (o, n)
        o += n
    CF["_n"] = o


_layout()


def make_consts():
    s_swa, s_nsa = _slopes()
    cb = np.zeros((128, CB["_n"]), np.float32)
    cf = np.zeros((128, CF["_n"]), np.float32)

    def putb(name, arr):
        c0, n = CB[name]
        arr = np.asarray(arr, np.float32).reshape(arr.shape[0], -1)
        assert arr.shape[1] == n, (name, arr.shape, n)
        cb[:arr.shape[0], c0:c0 + n] = arr

    def putf(name, arr):
        c0, n = CF[name]
        arr = np.asarray(arr, np.float32).reshape(arr.shape[0], -1)
        assert arr.shape[1] == n, (name, arr.shape, n)
        cf[:arr.shape[0], c0:c0 + n] = arr

    putb("identb", np.eye(128))
    putf("identf", np.eye(128))
    k = np.arange(SEQ)
    putb("esel", (k[None, :] // 64 == np.arange(32)[:, None]).astype(np.float32))
    c = np.arange(127)
    endc = 16 * c + 31
    putb("cmpmask", np.where(endc[:, None] <= k[None, :], 0.0, NEG))
    c0 = c * 16
    s0 = np.arange(32) * 64
    ov = np.clip(np.minimum(c0[:, None] + 32, s0[None, :] + 64) - np.maximum(c0[:, None], s0[None, :]), 0, None) / 32.0
    putb("ovp", np.concatenate([ov, np.ones((127, 1))], axis=1))
    kk = np.arange(128)
    putb("tri", (kk[:, None] <= kk[None, :]).astype(np.float32))
    def band_f(slopes, nwin, window):
        f = np.zeros((128, 8, nwin, 128))
        for h in range(8):
            for mi in range(nwin):
                m = nwin - 1 - mi
                dist = 128 * m + kk[None, :] - kk[:, None]
                e_ = -slopes[h] * dist - slopes[h] * (kk[:, None] - 127) / 2.0
                f[:, h, mi, :] = np.where((dist >= 0) & (dist < window), np.exp(np.minimum(e_, 80.0)), 0.0)
        return f
    putb("band_win", band_f(s_nsa, 3, 256))
    putb("band_swa", band_f(s_swa, 2, 128))
    tq = (np.arange(NT)[None, :] * 128 + kk[:, None])
    cur = tq // 64
    j = np.arange(32)[None, None, :]
    forced = ((j == 0) | (j == cur[:, :, None]) | (j == cur[:, :, None] - 1))
    putf("forced", np.where(forced, 1e9, 0.0))
    bc = np.zeros((128, 8, 4))
    for h in range(8):
        for Q in range(4):
            bc[:127, h, Q] = s_nsa[h] * (endc - (512 * Q + 256))
    putf("bias_cmp", bc)
    bsl = np.zeros((128, 8, 19))
    for h in range(8):
        for dk in range(-15, 4):
            bsl[:, h, dk + 15] = s_nsa[h] * (128 * dk + kk - 256)
    putf("bias_slc", bsl)
    bb = np.zeros((128, 16))
    for h in range(8):
        bb[:, h] = s_nsa[h] * (kk - 127) / 2.0
        bb[:, 8 + h] = s_swa[h] * (kk - 127) / 2.0
    putf("bias_band", bb)
    putf("sq_swa", np.zeros((128, 8)))
    return cb.astype(BF), cf.astype(np.float32)


C_QA, C_KCMP, C_VCMP, C_KSLC, C_VSLC, C_KWIN, C_VWIN, C_GNSA, C_QB, C_KB, C_VB, C_GM = (
    0, 512, 640, 768, 896, 1024, 1152, 1280, 1304, 1816, 1944, 2072)


def build_program(dbg=None, stop=None):
    dbg = dbg or []
    nc = bass.Bass("TRN2", target_bir_lowering=False)

    def din(name, shape, dt=F32):
        return nc.dram_tensor(name, list(shape), dt, kind="ExternalInput").ap()

    x_d = din("x", [SEQ, DM])
    constb_d = din("constb", [128, CB["_n"]], BF16)
    constf_d = din("constf", [128, CF["_n"]])
    gmix_d = din("norm_mix_g", [1, DM])
    win_d = din("w_in", [DM, 4120])
    posk_d = din("cmp_pos_k", [32, 64])
    w1k_d = din("cmp_w1_k", [2048, 256])
    w2k_d = din("cmp_w2_k", [256, 64])
    posv_d = din("cmp_pos_v", [32, 64])
    w1v_d = din("cmp_w1_v", [2048, 256])
    w2v_d = din("cmp_w2_v", [256, 64])
    sinks_d = din("sinks", [1, 8])
    wa_d = din("w_a", [512, DM])
    wb_d = din("w_b", [512, DM])
    wo_d = din("w_o", [DM, DM])
    gffn_d = din("norm_ffn_g", [1, DM])
    wgrp_d = din("w_group", [DM, 4])
    bgrp_d = din("b_group", [1, 4])
    wexp_d = din("w_expert", [DM, 32])
    bexp_d = din("b_expert", [1, 32])
    wg_d = din("w_gate_e", [32, DM, 256])
    wu_d = din("w_up_e", [32, DM, 256])
    wd_d = din("w_down_e", [32, 256, DM])
    gfin_d = din("norm_final_g", [1, DM])
    out_d = nc.dram_tensor("out", [SEQ, DM], F32, kind="ExternalOutput").ap()

    ARENA_BYTES = 212000
    with contextlib.ExitStack() as st:
        arena_h = st.enter_context(nc.sbuf_tensor("arena", [128, ARENA_BYTES // 2], BF16))
        ps_h = [st.enter_context(nc.psum_tensor("ps%d" % i, [128, 512], F32)) for i in range(8)]
        S = Sched(nc)
        S.debug = bool(globals().get('DEBUG_SCHED', False))
        ARENA = Buf(arena_h, [128, ARENA_BYTES // 2], BF16, "arena")

        def carve(name, fshape, dtype, at):
            fshape = list(fshape)
            n = int(np.prod(fshape))
            nb = n * _DT_SIZE[dtype]
            assert at % 4 == 0 and at + nb <= ARENA_BYTES, (name, at, nb)
            ap = arena_h[:, at // 2:(at + nb) // 2]
            if dtype == F32:
                ap = ap.bitcast(F32)
            if len(fshape) == 2:
                ap = ap.rearrange("p (a b) -> p a b", a=fshape[0])
            elif len(fshape) == 3:
                ap = ap.rearrange("p (a b c) -> p a b c", a=fshape[0], b=fshape[1])
            elif len(fshape) == 4:
                ap = ap.rearrange("p (a b c d) -> p a b c d", a=fshape[0], b=fshape[1], c=fshape[2])
            b = Buf(ap, [128] + fshape, dtype, name, base_byte=at, alias=ARENA)
            b.nbytes = nb
            return b

        class Region:
            def __init__(self, base, size):
                self.base, self.size, self.off = base, size, 0

            def take(self, name, fshape, dtype):
                self.off = (self.off + 3) // 4 * 4
                b = carve(name, fshape, dtype, self.base + self.off)
                self.off += b.nbytes
                assert self.off <= self.size, (name, self.off, self.size)
                return b

        PS = [Buf(p, [128, 512], F32, "ps%d" % i) for i, p in enumerate(ps_h)]
        for p_ in PS:
            p_.psum = True
        PSB = [Buf(p.bitcast(BF16), [128, 1024], BF16, "psb%d" % i, alias=PS[i]) for i, p in enumerate(ps_h)]

        def r3(view, pat, **kw):
            return view.with_ap(lambda a: a.rearrange(pat, **kw))

        _rb = [0]

        def _reg(size):
            r = Region(_rb[0], size)
            _rb[0] += size
            return r

        R_CONST = _reg(30720)
        R_HT = _reg(32768)
        R_Q = _reg(32768)
        R_K = _reg(32768)
        R_V = _reg(12544)
        R_SEL = _reg(8192)
        R_PT = _reg(12288)
        R_OA = _reg(32768)
        R_OB = _reg(16384)
        assert _rb[0] <= ARENA_BYTES, _rb[0]

        constb = R_CONST.take("constb", [CB["_n"]], BF16)
        constf = R_CONST.take("constf", [CF["_n"]], F32)
        gnsa = R_CONST.take("gnsa", [NT, 24], F32)
        ssq = R_CONST.take("ssq", [NT], F32)
        rstd = R_CONST.take("rstd", [NT], F32)
        kcT = R_CONST.take("kcT", [2, 128], BF16)
        vcp = R_CONST.take("vcp", [2, 65], BF16)
        sinkt = R_CONST.take("sinkt", [8], F32)
        smallf = R_CONST.take("smallf", [64], F32)

        def cbv(name, p0=0, p1=128):
            c0, n = CB[name]
            return constb, c0, n

        def CBs(name, rows=slice(0, 128), cols=None):
            c0, n = CB[name]
            if cols is None:
                cols = slice(0, n)
            return constb[rows, c0 + cols.start:c0 + cols.stop]

        def CFs(name, rows=slice(0, 128), cols=None):
            c0, n = CF[name]
            if cols is None:
                cols = slice(0, n)
            return constf[rows, c0 + cols.start:c0 + cols.stop]

        identb = CBs("identb")
        identf = CFs("identf")

        hT = R_HT.take("hT", [8, SEQ], BF16)
        qaT = R_Q.take("qaT", [4, SEQ], BF16)
        qbT = R_Q.take("qbT", [4, SEQ], BF16)
        kcmpT = R_K.take("kcmpT", [SEQ], BF16)
        vcmpT = R_K.take("vcmpT", [SEQ], BF16)
        kslc = R_K.take("kslc", [2, SEQ], BF16)
        kwin = R_K.take("kwin", [2, SEQ], BF16)
        kb = R_K.take("kb", [2, SEQ], BF16)
        vtm = R_V.take("vtm", [NT, 6, 65], BF16)
        selT = R_SEL.take("selT", [2, SEQ], BF16)
        NPT = 6
        PTb = [R_PT.take("pt%d" % i, [512], BF16) for i in range(NPT)]
        evs = R_PT.take("evs", [512], F32)
        oa = R_OA.take("oa", [NT, 8, 64], F32)
        ob = R_OB.take("ob", [NT, 8, 64], BF16)

        debug_outs = []

        def tap(name, buf_view, shape, dt=F32):
            if name not in dbg:
                return
            d = nc.dram_tensor("dbg_" + name, list(shape), dt, kind="ExternalOutput").ap()
            S.dma(d, buf_view, is_out=True)

        S.dma(constb[:, :], constb_d)
        S.dma(constf[:, :], constf_d)

        xall = Buf(arena_h[:, R_Q.base // 2:(R_Q.base + 65536) // 2].bitcast(F32).rearrange("p (t d) -> p t d", t=NT),
                   [128, NT, DM], F32, "xall", base_byte=R_Q.base, alias=ARENA)
        PW = Region(R_OA.base, R_OA.size)
        wst = [PW.take("wst%d" % i, [8, 128], BF16) for i in range(16)]
        P0 = Region(R_OB.base, R_OB.size)
        wtm = P0.take("wtm", [8, 408], BF16)
        gbc = P0.take("gbc", [DM], F32)
        hb = [P0.take("hb%d" % i, [DM], BF16) for i in range(2)]
        PJ = Region(R_SEL.base, R_SEL.size + R_PT.size)
        w1k_buf = PJ.take("w1k", [32, 256], BF16)
        junk = PJ.take("junk", [DM], BF16)

        win_v = win_d.rearrange("(kc p) n -> p kc n", p=128)
        S.dma(gbc[:, :], gmix_d.partition_broadcast(128))

        def xtile(t):
            a_, b_ = t // 4, t % 4
            lo = (4 * b_) * 4096 + a_ * 1024
            hi = (4 * b_ + 3) * 4096 + a_ * 1024 + 1024
            ap = xall.h.rearrange("p s (q c) -> p s q c", q=4)[:, 4 * b_:4 * b_ + 4, a_, :]
            boxes = [(0, 128, R_Q.base + (4 * b_ + j) * 4096 + a_ * 1024, R_Q.base + (4 * b_ + j) * 4096 + a_ * 1024 + 1024)
                     for j in range(4)]
            return View(ARENA, ap, (0, 128, R_Q.base + lo, R_Q.base + hi), boxes)

        for t in range(NT):
            S.dma(xtile(t), x_d[t * 128:(t + 1) * 128, :].rearrange("p (j c) -> p j c", j=4))
        fm = []
        for c in range(4):
            fm.append((lambda tb, c=c: qaT[:, c, tb * 512:(tb + 1) * 512], [(C_QA + c * 128, 128)], 0.125))
        fm.append((lambda tb: kcmpT[:, tb * 512:(tb + 1) * 512], [(C_KCMP, 128)], 1.0))
        fm.append((lambda tb: vcmpT[:, tb * 512:(tb + 1) * 512], [(C_VCMP, 128)], 1.0))
        for (dst, c0) in ((kslc, C_KSLC), (kwin, C_KWIN)):
            for g in range(2):
                fm.append((lambda tb, dst=dst, g=g: dst[:, g, tb * 512:(tb + 1) * 512],
                           [(c0 + g * 64, 64), (c0 + g * 64, 64)], 1.0))
        for c in range(4):
            fm.append((lambda tb, c=c: qbT[:, c, tb * 512:(tb + 1) * 512], [(C_QB + c * 128, 128)], 0.125))
        for g in range(2):
            fm.append((lambda tb, g=g: kb[:, g, tb * 512:(tb + 1) * 512],
                       [(C_KB + g * 64, 64), (C_KB + g * 64, 64)], 1.0))
        for ci, (dstf, pieces, scale) in enumerate(fm):
            o = 0
            for (c0, n) in pieces:
                S.dma(wst[ci][:, :, o:o + n], win_v[:, :, c0:c0 + n], eng="pool")
                o += n
        for i, c0 in enumerate((C_VSLC, C_VWIN, C_VB)):
            S.dma(wtm[:, :, i * 128:(i + 1) * 128], win_v[:, :, c0:c0 + 128], eng="pool")
        S.dma(wtm[:, :, 384:408], win_v[:, :, C_GNSA:C_GNSA + 24], eng="pool")
        w1kv_ = w1k_d.rearrange("(l d) h -> d l h", d=64)
        S.dma(w1k_buf[0:64, :, :], w1kv_, eng="pool")
        S.dma(w1k_buf[64:128, :, :], w1kv_, eng="pool")

        def p0_group(t4):
            j3 = r3(junk[:, :], "p (j c) -> p j c", j=4)
            for t in range(t4, t4 + 4):
                S.act(j3, xtile(t), AF.Square, accum=ssq[:, t:t + 1])
            S.ts(rstd[:, t4:t4 + 4], ssq[:, t4:t4 + 4], 1.0 / DM, 1e-6, ALU.mult, ALU.add)
            S.act(rstd[:, t4:t4 + 4], rstd[:, t4:t4 + 4], AF.Sqrt)
            S.recip(rstd[:, t4:t4 + 4], rstd[:, t4:t4 + 4])
            for t in range(t4, t4 + 4):
                h = hb[t % 2]
                S.stt(r3(h[:, :], "p (j c) -> p j c", j=4), xtile(t), rstd[:, t:t + 1],
                      r3(gbc[:, :], "p (j c) -> p j c", j=4), ALU.mult, ALU.mult)
                pb = PSB[t % 2]
                for c in range(8):
                    S.transpose(pb[:, c * 128:(c + 1) * 128], h[:, c * 128:(c + 1) * 128], identb)
                S.copy(hT[:, :, t * 128:(t + 1) * 128], r3(pb[:, 0:1024], "p (c n) -> p c n", c=8),
                       eng=("act" if t % 2 else "dve"))

        pj = {"nev": 0}

        def proj_block(tb):
            for ci, (dstf, pieces, scale) in enumerate(fm):
                w = wst[ci]
                p = PS[2 + (pj["nev"] % 4)]
                for kc in range(8):
                    S.mm(p[:, :], w[:, kc, :], hT[:, kc, tb * 512:(tb + 1) * 512], start=(kc == 0), stop=(kc == 7))
                if pj["nev"] % 2 == 0:
                    S.act(dstf(tb), p[:, :], AF.Copy, scale=scale)
                else:
                    S.ts(dstf(tb), p[:, :], scale, None, ALU.mult)
                pj["nev"] += 1
            for t in range(4 * tb, 4 * tb + 4):
                p = PS[6 + t % 2]
                for kc in range(8):
                    S.mm(p[:, 0:408], hT[:, kc, t * 128:(t + 1) * 128], wtm[:, kc, :], start=(kc == 0), stop=(kc == 7))
                S.copy(vtm[:, t, :, 0:64], r3(p[:, 0:384], "p (s d) -> p s d", s=6), eng="dve")
                S.act(gnsa[:, t, :], p[:, 384:408], AF.Sigmoid)

        S.memset(vtm[:, :, :, 64:65], 1.0)
        p0_group(0)
        p0_group(4)
        proj_block(0)
        p0_group(8)
        proj_block(1)
        p0_group(12)
        proj_block(2)
        proj_block(3)
        tap("hT", hT[:, :, :], [128, 8, SEQ], BF16)
        if stop == "p0":
            S.emit()
            return nc

        for nm, b, sh in (("qaT", qaT, [128, 4, SEQ]), ("qbT", qbT, [128, 4, SEQ]), ("kslc", kslc, [128, 2, SEQ]),
                          ("kcmpT", kcmpT, [128, SEQ]), ("vtm", vtm, [128, NT, 6, 65])):
            tap(nm, b[(slice(None),) * len(sh)], sh, BF16)
        tap("gnsa", gnsa[:, :, :], [128, NT, 24], F32)
        if stop == "proj":
            S.emit()
            return nc

        PC = Region(R_OA.base, R_OA.size + R_OB.size)
        w1d = {"k": w1k_buf, "v": PC.take("w1v", [32, 256], BF16)}
        w2kd = PC.take("w2kd", [2, 128], BF16)
        w2v = PC.take("w2v", [2, 64], BF16)
        posT = {"k": PC.take("posTk", [32], BF16), "v": PC.take("posTv", [32], BF16)}
        posb = {"k": PC.take("posbk", [2], F32), "v": PC.take("posbv", [2], F32)}
        hidT = {"k": PC.take("hidTk", [2, 2, 128], BF16), "v": PC.take("hidTv", [2, 2, 128], BF16)}
        for X, w1_d, pos_d in (("k", w1k_d, posk_d), ("v", w1v_d, posv_d)):
            if X == "v":
                w1v_ = w1_d.rearrange("(l d) h -> d l h", d=64)
                S.dma(w1d[X][0:64, :, :], w1v_, eng="pool")
                S.dma(w1d[X][64:128, :, :], w1v_, eng="pool")
            S.dma(posT[X][0:64, :], pos_d.rearrange("l d -> d l"), eng="pool", allow_slow_non_contiguous=True)
        w2k_v = w2k_d.rearrange("(hc p) d -> p hc d", p=128)
        S.dma(w2kd[:, :, 0:64], w2k_v, eng="pool")
        S.dma(w2kd[:, :, 64:128], w2k_v, eng="pool")
        S.dma(w2v[:, :, :], w2v_d.rearrange("(hc p) d -> p hc d", p=128), eng="pool")
        S.memset(vcp[:, :, 64:65], 1.0)
        for X, srcT in (("k", kcmpT), ("v", vcmpT)):
            pp = PS[0]
            for hc in range(2):
                for l in range(32):
                    S.mm(pp[:, hc:hc + 1], w1d[X][0:64, l, hc * 128:(hc + 1) * 128], posT[X][0:64, l:l + 1],
                         start=(l == 0), stop=(l == 31))
            S.copy(posb[X][:, :], pp[:, 0:2])
            for hc in range(2):
                pg_ = [PS[1 + 2 * hc], PS[2 + 2 * hc]]
                for l in range(32):
                    for g in range(2):
                        S.mm(pg_[g][:, 0:127], w1d[X][64 * g:64 * g + 64, l, hc * 128:(hc + 1) * 128],
                             srcT[64 * g:64 * g + 64, l:l + 16 * 126 + 1:16], start=(l == 0), stop=(l == 31))
                for g in range(2):
                    S.act(hidT[X][:, g, hc, 0:127], pg_[g][:, 0:127], AF.Silu, bias=posb[X][:, hc:hc + 1])
        for g in range(2):
            p = PS[5 + g]
            for hc in range(2):
                S.mm(p[:, g * 128:g * 128 + 127], w2kd[:, hc, :], hidT["k"][:, g, hc, 0:127], start=(hc == 0), stop=(hc == 1))
            S.copy(kcT[:, g, 0:127], p[:, g * 128:g * 128 + 127])
        for g in range(2):
            p = PS[6 + g]
            for hc in range(2):
                S.mm(p[0:127, g * 64:(g + 1) * 64], hidT["v"][:, g, hc, 0:127], w2v[:, hc, :], start=(hc == 0), stop=(hc == 1))
            S.copy(vcp[0:127, g, 0:64], p[0:127, g * 64:(g + 1) * 64], eng="act")
        RG0 = Region(R_K.base, 8192)
        wgs = [RG0.take("wgs%d" % i, [8, 256], BF16) for i in range(2)]
        wgs += [None, None]
        gpieces = {}

        def load_gate_piece(fp):
            for ab in range(2):
                w = wgs[(2 * fp + ab) % 4]
                c0 = C_GM + ab * 1024 + fp * 256
                S.dma(w[:, :, :], win_v[:, :, c0:c0 + 256], eng="pool")
                gpieces[(fp, ab)] = w

        load_gate_piece(0)
        tap("kcT", kcT[:, :, 0:127], [128, 2, 127], BF16)
        tap("vcp", vcp[0:127, :, :], [127, 2, 65], BF16)
        if stop == "cmpmlp":
            S.emit()
            return nc

        sk = smallf[:, 0:8]
        S.dma(sk, sinks_d.partition_broadcast(128))
        S.tt(sk, sk, CFs("sq_swa"), ALU.add)
        S.act(sinkt[:, :], sk, AF.Exp)

        imp_sb = R_PT.take("imp_sb", [4, 2, 32], F32)
        selb = R_PT.take("selb", [4, 2, 32], BF16)
        Msb = [R_PT.take("msb%d" % i, [512], BF16) for i in range(2)]
        top8 = R_PT.take("top8", [8], F32)
        ev_rz = evs[:, 0:4]
        ev_coef = evs[:, 4:8]
        ev_rzi = evs[:, 8:12]
        ev_tmp = evs[:, 64:64 + 256]
        ev_tmp2 = evs[:, 320:320 + 128]
        S.memset(selT[:, :, :], 0.0)

        st_banks = [0, 1, 2, 3]
        cnt = {"st": 0, "pt": 0}

        def next_st():
            b_ = PS[st_banks[cnt["st"] % 4]]
            cnt["st"] += 1
            return b_

        def next_pt():
            b_ = PTb[cnt["pt"] % NPT]
            cnt["pt"] += 1
            return b_

        def accv(p, u0, u1, d0, d1):
            v = p[:, u0 * 65:(u1 - 1) * 65 + d1]
            ap = p.h[:, 0:260].rearrange("p (u d) -> p u d", u=4)[:, u0:u1, d0:d1]
            return View(v.buf, ap, v.box)

        def acc1(p, u):
            return accv(p, u, u + 1, 0, 65).with_ap(lambda a: a[:, 0, :])

        def evac_nsa(p, Q, h, br, first):
            z = accv(p, 0, 4, 64, 65)
            S.ts(ev_rz.with_ap(lambda a: a.unsqueeze(2)), z, 1e-30, None, ALU.max)
            S.recip(ev_rz, ev_rz)
            gate = gnsa[:, 4 * Q:4 * Q + 4, h * 3 + br]
            S.tt(ev_coef, ev_rz, gate, ALU.mult)
            cb_ = ev_coef.with_ap(lambda a: a.unsqueeze(2).broadcast_to([128, 4, 64]))
            o = oa[:, 4 * Q:4 * Q + 4, h, :]
            if first:
                S.tt(o, accv(p, 0, 4, 0, 64), cb_, ALU.mult)
            else:
                t3 = r3(ev_tmp, "p (u d) -> p u d", u=4)
                S.tt(t3, accv(p, 0, 4, 0, 64), cb_, ALU.mult)
                S.tt(o, o, t3, ALU.add, eng="pool")

        HALF = ((0, 64), (64, 128))

        def do_cmp(Q):
            qs = slice(Q * 512, (Q + 1) * 512)
            pend = []

            def finish(step):
                c, pts = step
                g = c // 2
                hs = (2 * c, 2 * c + 1)
                for e in range(2):
                    accp = PS[4 + e]
                    impp = PS[6 + e]
                    pt = pts[e]
                    for u in range(4):
                        S.mm(acc1(accp, u), pt[0:127, u * 128:(u + 1) * 128], vcp[0:127, g, :],
                             start=(u == 0), stop=True, skip_group_check=True)
                    for u in range(4):
                        S.mm(impp[:, u * 33:(u + 1) * 33], pt[0:127, u * 128:(u + 1) * 128], CBs("ovp", slice(0, 127)),
                             start=(u == 0), stop=True, skip_group_check=True)
                for e in range(2):
                    h = hs[e]
                    accp = PS[4 + e]
                    impp = PS[6 + e]
                    evac_nsa(accp, Q, h, 0, True)
                    zi = View(impp, impp.h[:, 0:132].rearrange("p (u d) -> p u d", u=4)[:, :, 32:33], impp[:, 0:132].box)
                    S.ts(ev_rzi.with_ap(lambda a: a.unsqueeze(2)), zi, 1e-30, None, ALU.max)
                    S.recip(ev_rzi, ev_rzi)
                    rb = ev_rzi.with_ap(lambda a: a.unsqueeze(2).broadcast_to([128, 4, 32]))
                    iv = View(impp, impp.h[:, 0:132].rearrange("p (u d) -> p u d", u=4)[:, :, 0:32], impp[:, 0:132].box)
                    if h % 4 == 0:
                        S.tt(imp_sb[:, :, g, :], iv, rb, ALU.mult)
                    else:
                        t3 = r3(ev_tmp2, "p (u d) -> p u d", u=4)
                        S.tt(t3, iv, rb, ALU.mult)
                        S.tt(imp_sb[:, :, g, :], imp_sb[:, :, g, :], t3, ALU.add, eng="pool")

            for c in range(4):
                g = c // 2
                hs = (2 * c, 2 * c + 1)
                stp = [next_st(), next_st()]
                for e in range(2):
                    lo, hi = HALF[e]
                    S.mm(stp[e][0:127, :], kcT[lo:hi, g, 0:127], qaT[lo:hi, c, qs], start=True, stop=False)
                for e in range(2):
                    S.mm(stp[e][0:127, :], CBs("identb", slice(0, 127), slice(0, 127)), CBs("cmpmask", slice(0, 127), qs),
                         start=False, stop=True)
                pts = [next_pt(), next_pt()]
                for e in range(2):
                    h = hs[e]
                    S.act(pts[e][0:127, :], stp[e][0:127, :], AF.Exp,
                          bias=CFs("bias_cmp", slice(0, 127), slice(h * 4 + Q, h * 4 + Q + 1)))
                pend.append((c, pts))
                if len(pend) > 1:
                    finish(pend.pop(0))
            while pend:
                finish(pend.pop(0))

        def do_select(Q):
            c0, _ = CF["forced"]
            fv = View(constf.alias, constf.h[:, c0 + 4 * Q * 32:c0 + (4 * Q + 4) * 32].rearrange("p (u j) -> p u j", u=4)
                      .unsqueeze(2).broadcast_to([128, 4, 2, 32]), constf[:, c0 + 4 * Q * 32:c0 + (4 * Q + 4) * 32].box)
            S.tt(imp_sb[:, :, :, :], imp_sb[:, :, :, :], fv, ALU.max)
            for g in range(2):
                pb = PSB[6 + g]
                for u in range(4):
                    S.max8(top8[:, :], imp_sb[:, u, g, :])
                    S.ts(selb[:, u, g, :], imp_sb[:, u, g, :], top8[:, 7:8], None, ALU.is_ge)
                    S.transpose(pb[0:32, u * 128:(u + 1) * 128], selb[:, u, g, :], identb)
                S.copy(selT[0:32, g, Q * 512:(Q + 1) * 512], pb[0:32, 0:512], eng="act")

        msb_cnt = [0]

        def do_slc(Q):
            nk = 4 * Q + 4
            for g in range(2):
                cs = (2 * g, 2 * g + 1)
                accs = {cs[0]: [PS[4], PS[5]], cs[1]: [PS[6], PS[7]]}
                pend = []

                def pv(kt, c, pts):
                    r = kt - 4 * Q
                    for e in range(2):
                        for u in range(max(r, 0), 4):
                            S.mm(acc1(accs[c][e], u), pts[e][:, u * 128:(u + 1) * 128],
                                 vtm[:, kt, 0 * 2 + g, :], start=(kt == 0 and u == 0), stop=True, skip_group_check=True)

                def make_mask(kt):
                    r = kt - 4 * Q
                    c0 = 128 * r if r > 0 else 0
                    mp = next_st()
                    S.mm(mp[:, c0:512], CBs("esel", slice(0, 128), slice(kt * 128, (kt + 1) * 128)),
                         selT[:, g, Q * 512 + c0:(Q + 1) * 512], start=True, stop=True)
                    ms = Msb[msb_cnt[0] % 2]
                    msb_cnt[0] += 1
                    S.act(ms[:, c0:512], mp[:, c0:512], AF.Copy)
                    if r >= 0:
                        S.tt(ms[:, c0:c0 + 128], ms[:, c0:c0 + 128], CBs("tri"), ALU.mult, eng="pool")
                    return ms

                ms_next = make_mask(0)
                for kt in range(nk):
                    r = kt - 4 * Q
                    c0 = 128 * r if r > 0 else 0
                    ms = ms_next
                    if kt + 1 < nk:
                        ms_next = make_mask(kt + 1)
                    for c in cs:
                        hs = (2 * c, 2 * c + 1)
                        stp = [next_st(), next_st()]
                        for e in range(2):
                            lo, hi = HALF[e]
                            S.mm(stp[e][:, c0:512], kslc[lo:hi, g, kt * 128:(kt + 1) * 128],
                                 qaT[lo:hi, c, Q * 512 + c0:(Q + 1) * 512], start=True, stop=True)
                        pts = [next_pt(), next_pt()]
                        for e in range(2):
                            h = hs[e]
                            bcol = h * 19 + (kt - 4 * Q + 15)
                            S.act(pts[e][:, c0:512], stp[e][:, c0:512], AF.Exp,
                                  bias=CFs("bias_slc", slice(0, 128), slice(bcol, bcol + 1)))
                            S.tt(pts[e][:, c0:512], pts[e][:, c0:512], ms[:, c0:512], ALU.mult)
                        pend.append((kt, c, pts))
                        if len(pend) > 1:
                            pv(*pend.pop(0))
                while pend:
                    pv(*pend.pop(0))
                for c in cs:
                    for e in range(2):
                        evac_nsa(accs[c][e], Q, 2 * c + e, 1, False)

        band_cnt = [0]

        def do_band(Q, which):
            nwin = 3 if which == "win" else 2
            if which == "win":
                qT_, kT_, vbase, band = qaT, kwin, 1 * 2, "band_win"
            else:
                qT_, kT_, vbase, band = qbT, kb, 2 * 2, "band_swa"
            bc0, _ = CB[band]
            pend = []

            def finish(step):
                c, u, js, pts, accs, i = step
                g = c // 2
                hs = (2 * c, 2 * c + 1)
                for e in range(2):
                    for j in js:
                        mi = j - (i - nwin + 1)
                        S.mm(acc1(accs[e], u), pts[e][:, mi * 128:(mi + 1) * 128],
                             vtm[:, j, vbase + g, :], start=(u == 0 and j == js[0]), stop=True, skip_group_check=True)
                if u == 3:
                    for e in range(2):
                        h = hs[e]
                        if which == "win":
                            evac_nsa(accs[e], Q, h, 2, False)
                        else:
                            z = accv(accs[e], 0, 4, 64, 65)
                            S.ts(ev_rz.with_ap(lambda a: a.unsqueeze(2)), z, sinkt[:, h:h + 1], None, ALU.add)
                            S.recip(ev_rz, ev_rz)
                            cb_ = ev_rz.with_ap(lambda a: a.unsqueeze(2).broadcast_to([128, 4, 64]))
                            S.tt(ob[:, 4 * Q:4 * Q + 4, h, :], accv(accs[e], 0, 4, 0, 64), cb_, ALU.mult)

            for c in range(4):
                g = c // 2
                hs = (2 * c, 2 * c + 1)
                ab = 4 + 2 * (band_cnt[0] % 2)
                band_cnt[0] += 1
                accs = [PS[ab], PS[ab + 1]]
                for u in range(4):
                    i = 4 * Q + u
                    js = [j for j in range(i - nwin + 1, i + 1) if j >= 0]
                    stp = [next_st(), next_st()]
                    for j in js:
                        mi = j - (i - nwin + 1)
                        for e in range(2):
                            lo, hi = HALF[e]
                            S.mm(stp[e][:, mi * 128:(mi + 1) * 128], kT_[lo:hi, g, j * 128:(j + 1) * 128],
                                 qT_[lo:hi, c, i * 128:(i + 1) * 128], start=(j == js[0]), stop=True, skip_group_check=True)
                    mi0 = js[0] - (i - nwin + 1)
                    pts = [next_pt(), next_pt()]
                    for e in range(2):
                        h = hs[e]
                        hcol = h if which == "win" else 8 + h
                        S.act(pts[e][:, mi0 * 128:nwin * 128], stp[e][:, mi0 * 128:nwin * 128], AF.Exp,
                              bias=CFs("bias_band", slice(0, 128), slice(hcol, hcol + 1)))
                        boff = bc0 + (h * nwin + mi0) * 128
                        S.tt(pts[e][:, mi0 * 128:nwin * 128], pts[e][:, mi0 * 128:nwin * 128],
                             constb[:, boff:boff + (nwin - mi0) * 128], ALU.mult, eng=("pool" if (which == "win" and e == 0) else "dve"))
                    pend.append((c, u, js, pts, accs, i))
                    if len(pend) > 1:
                        finish(pend.pop(0))
            while pend:
                finish(pend.pop(0))

        for Q in range(4):
            do_cmp(Q)
            do_select(Q)
            do_slc(Q)
            do_band(Q, "win")
            do_band(Q, "swa")
        tap("selT", selT[0:32, :, :], [32, 2, SEQ], BF16)
        tap("selT2", selT[64:96, :, :], [32, 2, SEQ], BF16)
        tap("oa", oa[:, :, :, :], [128, NT, 8, 64], F32)
        tap("ob", ob[:, :, :, :], [128, NT, 8, 64], BF16)
        if stop == "attn":
            S.emit()
            return nc

        oaT = Buf(qaT.h, qaT.shape, BF16, "oaT", base_byte=qaT.base_byte, alias=ARENA)
        obT = Buf(qbT.h, qbT.shape, BF16, "obT", base_byte=qbT.base_byte, alias=ARENA)
        RM = Region(R_K.base, R_K.size + R_V.size + R_SEL.size + R_PT.size)
        RM.take("wgs01_reserved", [2, 8, 256], BF16)
        wa = RM.take("wa", [4, DM], BF16)
        wbm = RM.take("wbm", [4, DM], BF16)
        wo = RM.take("wo", [8, DM], BF16)
        wgs[2] = RM.take("wgs2", [8, 256], BF16)
        wgs[3] = RM.take("wgs3", [8, 256], BF16)
        oab = [RM.take("oab%d" % i, [512], BF16) for i in range(2)]
        sg = [RM.take("sg%d" % i, [512], F32) for i in range(2)]
        tA = RM.take("tA", [512], F32)
        tB = RM.take("tB", [512], F32)
        mixT = Buf(arena_h[:, R_OA.base // 2:(R_OA.base + 32768) // 2].rearrange("p (a b) -> p a b", a=8), [128, 8, SEQ], BF16,
                   "mixT", base_byte=R_OA.base, alias=ARENA)
        RX = Region(R_OB.base, R_OB.size)
        xr = [RX.take("xr%d" % i, [DM], F32) for i in range(2)]

        sqjunk = Buf(arena_h[:, sg[0].base_byte // 2:sg[0].base_byte // 2 + 1024], [128, DM], BF16, "sqjunk",
                     base_byte=sg[0].base_byte, alias=ARENA)
        S.dma(wa[:, :, :], wa_d.rearrange("(kc p) n -> p kc n", p=128), eng="pool")
        S.dma(wbm[:, :, :], wb_d.rearrange("(kc p) n -> p kc n", p=128), eng="pool")
        S.dma(wo[:, :, :], wo_d.rearrange("(kc p) n -> p kc n", p=128), eng="pool")
        obv = Buf(ob.h.rearrange("p t h d -> p t (h d)"), [128, NT, 512], BF16, "obv", base_byte=ob.base_byte, alias=ARENA)
        for t in range(NT):
            o_ = oab[t % 2]
            S.copy(o_[:, :], r3(oa[:, t, :, :], "p h d -> p (h d)"), eng="act")
            pb = PSB[t % 2]
            for c in range(4):
                S.transpose(pb[:, c * 128:(c + 1) * 128], o_[:, c * 128:(c + 1) * 128], identb)
            for c in range(4):
                S.transpose(pb[:, 512 + c * 128:512 + (c + 1) * 128], obv[:, t, c * 128:(c + 1) * 128], identb)
            S.copy(oaT[:, :, t * 128:(t + 1) * 128], r3(pb[:, 0:512], "p (c n) -> p c n", c=4), eng="dve")
            S.copy(obT[:, :, t * 128:(t + 1) * 128], r3(pb[:, 512:1024], "p (c n) -> p c n", c=4), eng="dve")
        n = 0
        for fo in range(8):
            if fo % 2 == 0:
                if fo // 2 + 1 < 4:
                    load_gate_piece(fo // 2 + 1)
            for tb in range(4):
                ts_ = slice(tb * 512, (tb + 1) * 512)
                res = []
                for ab, (wproj, oT, tdst) in enumerate(((wa, oaT, tA), (wbm, obT, tB))):
                    pg = PS[2 + n % 2]
                    pa = PS[4 + n % 2]
                    n += 1
                    for kc in range(8):
                        S.mm(pg[:, :], gpieces[(fo // 2, ab)][:, kc, (fo % 2) * 128:(fo % 2 + 1) * 128], hT[:, kc, ts_],
                             start=(kc == 0), stop=(kc == 7))
                    for ki in range(4):
                        S.mm(pa[:, :], wproj[:, ki, fo * 128:(fo + 1) * 128], oT[:, ki, ts_], start=(ki == 0), stop=(ki == 3))
                    s_ = sg[ab]
                    S.act(s_[:, :], pg[:, :], AF.Sigmoid)
                    S.tt(tdst[:, :], pa[:, :], s_[:, :], ALU.mult)
                S.tt(mixT[:, fo, ts_], tA[:, :], tB[:, :], ALU.add, eng="pool")
        x1 = Buf(arena_h[:, R_HT.base // 2:(R_HT.base + 65536) // 2].bitcast(F32).rearrange("p (t d) -> p t d", t=NT),
                 [128, NT, DM], F32, "x1", base_byte=R_HT.base, alias=ARENA)
        for t in range(NT):
            xs = xr[t % 2]
            S.dma(xs[:, :], x_d[t * 128:(t + 1) * 128, :])
            for dh in range(2):
                p = PS[6 + dh]
                for fo in range(8):
                    S.mm(p[:, :], mixT[:, fo, t * 128:(t + 1) * 128], wo[:, fo, dh * 512:(dh + 1) * 512],
                         start=(fo == 0), stop=(fo == 7))
                S.tt(x1[:, t, dh * 512:(dh + 1) * 512], p[:, :], xs[:, dh * 512:(dh + 1) * 512], ALU.add)
            S.act(sqjunk[:, :], x1[:, t, :], AF.Square, accum=ssq[:, t:t + 1])
        tap("x1", x1[:, :, :], [128, NT, DM], F32)
        if stop == "merge":
            S.emit()
            return nc

        h2T = Buf(arena_h[:, R_OA.base // 2:(R_OA.base + 32768) // 2].rearrange("p (a b) -> p a b", a=8), [128, 8, SEQ], BF16,
                  "h2T", base_byte=R_OA.base, alias=ARENA)
        RE = Region(R_K.base, R_K.size + R_V.size + R_SEL.size + R_PT.size)
        Wt = RE.take("Wt", [NT, 32], F32)
        wgu = [RE.take("wgu%d" % i, [2, 8, 256], BF16) for i in range(2)]
        wdn = [RE.take("wdn%d" % i, [2, DM], BF16) for i in range(3)]
        AT = [RE.take("AT%d" % i, [2, SEQ], BF16) for i in range(2)]
        sGs = [RE.take("sG%d" % i, [512], BF16) for i in range(2)]
        uSs = [RE.take("uS%d" % i, [512], BF16) for i in range(2)]
        RR = Region(R_OB.base, R_OB.size)
        gbc2 = RR.take("gbc2", [DM], F32)
        h2f_a = RR.take("h2f", [DM], F32)
        h2b_a = RR.take("h2b", [DM], BF16)
        h2T32_a = RR.take("h2T32", [8, 128], F32)
        wr32 = RR.take("wr32", [8, 36], F32)
        RR2 = Region(AT[0].base_byte, 16384)
        h2f_b = RR2.take("h2f_b", [DM], F32)
        h2b_b = RR2.take("h2b_b", [DM], BF16)
        h2T32_b = RR2.take("h2T32_b", [8, 128], F32)
        h2b = h2b_a
        lgTs = [RR2.take("lgT%d" % i, [128], F32) for i in range(2)]
        RS = RE
        Lg = RS.take("Lg", [NT, 36], F32)
        bias36 = RS.take("bias36", [36], F32)
        ss2 = RS.take("ss2", [NT], F32)
        rstd2 = RS.take("rstd2", [NT], F32)
        rt = [RS.take("rt%d" % i, [NT], F32) for i in range(6)]
        gm4 = RS.take("gm4", [NT, 4], F32)
        ge4 = RS.take("ge4", [NT, 4], F32)
        m1 = RS.take("m1", [NT, 32], F32)
        m2 = RS.take("m2", [NT, 32], F32)
        em = RS.take("em", [NT, 32], F32)

        S.dma(gbc2[:, :], gffn_d.partition_broadcast(128))
        S.dma(wr32[:, :, 0:4], wgrp_d.rearrange("(kc p) n -> p kc n", p=128))
        S.dma(wr32[:, :, 4:36], wexp_d.rearrange("(kc p) n -> p kc n", p=128))
        S.dma(bias36[:, 0:4], bgrp_d.partition_broadcast(128))
        S.dma(bias36[:, 4:36], bexp_d.partition_broadcast(128))
        S.ts(rstd2[:, :], ssq[:, :], 1.0 / DM, 1e-6, ALU.mult, ALU.add)
        S.act(rstd2[:, :], rstd2[:, :], AF.Sqrt)
        S.recip(rstd2[:, :], rstd2[:, :])
        for t in range(NT):
            h2f, h2b, h2T32 = (h2f_a, h2b_a, h2T32_a) if t % 2 == 0 else (h2f_b, h2b_b, h2T32_b)
            S.stt(h2f[:, :], x1[:, t, :], rstd2[:, t:t + 1], gbc2[:, :], ALU.mult, ALU.mult)
            S.copy(h2b[:, :], h2f[:, :], eng="act")
            pb = PSB[t % 2]
            for c in range(8):
                S.transpose(pb[:, c * 128:(c + 1) * 128], h2b[:, c * 128:(c + 1) * 128], identb)
            S.copy(h2T[:, :, t * 128:(t + 1) * 128], r3(pb[:, 0:1024], "p (c n) -> p c n", c=8), eng="dve")
            for half in range(2):
                pf = PS[(2 if t % 2 == 0 else 6) + half]
                for c in range(4):
                    cc = half * 4 + c
                    S.transpose(pf[:, c * 128:(c + 1) * 128], h2f[:, cc * 128:(cc + 1) * 128], identf)
                S.copy(h2T32[:, half * 4:(half + 1) * 4, :], r3(pf[:, :], "p (c n) -> p c n", c=4), eng="act")
            pl = PS[4 + t % 2]
            for c in range(8):
                S.mm(pl[0:36, 0:128], wr32[:, c, :], h2T32[:, c, :], start=(c == 0), stop=(c == 7))
            lgT = lgTs[t % 2]
            S.copy(lgT[0:36, :], pl[0:36, 0:128], eng="act")
            S.transpose(pl[:, 256:292], lgT[0:36, :], CFs("identf", slice(0, 36), slice(0, 36)))
            S.tt(Lg[:, t, :], pl[:, 256:292], bias36[:, :], ALU.add)
        gl = Lg[:, :, 0:4]
        el = Lg[:, :, 4:36]
        gmax, gsum, top1, top2, w1_, a1 = rt
        S.reduce(gmax[:, :], gl, ALU.max)
        gmb = gmax[:, :].with_ap(lambda a: a.unsqueeze(2).broadcast_to([128, NT, 4]))
        S.tt(gm4[:, :, :], gl, gmb, ALU.is_equal)
        S.tt(ge4[:, :, :], gl, gmb, ALU.subtract)
        S.act(ge4[:, :, :], ge4[:, :, :], AF.Exp)
        S.reduce(gsum[:, :], ge4[:, :, :], ALU.add)
        S.recip(gsum[:, :], gsum[:, :])
        S.ts(gm4[:, :, :], gm4[:, :, :], -1.0, 1e30, ALU.add, ALU.mult)
        pen = gm4[:, :, :].with_ap(lambda a: a.unsqueeze(3).broadcast_to([128, NT, 4, 8]))
        S.tt(r3(em[:, :, :], "p t (g e) -> p t g e", g=4), r3(el, "p t (g e) -> p t g e", g=4), pen, ALU.add)
        S.reduce(top1[:, :], em[:, :, :], ALU.max)
        S.tt(m1[:, :, :], em[:, :, :], top1[:, :].with_ap(lambda a: a.unsqueeze(2).broadcast_to([128, NT, 32])), ALU.is_equal)
        S.stt(em[:, :, :], m1[:, :, :], -1e30, em[:, :, :], ALU.mult, ALU.add)
        S.reduce(top2[:, :], em[:, :, :], ALU.max)
        S.tt(m2[:, :, :], em[:, :, :], top2[:, :].with_ap(lambda a: a.unsqueeze(2).broadcast_to([128, NT, 32])), ALU.is_equal)
        S.tt(w1_[:, :], top2[:, :], top1[:, :], ALU.subtract)
        S.act(w1_[:, :], w1_[:, :], AF.Exp)
        S.ts(w1_[:, :], w1_[:, :], 1.0, None, ALU.add)
        S.recip(w1_[:, :], w1_[:, :])
        S.tt(a1[:, :], gsum[:, :], w1_[:, :], ALU.mult)
        S.tt(top2[:, :], gsum[:, :], a1[:, :], ALU.subtract)
        S.tt(m1[:, :, :], m1[:, :, :], a1[:, :].with_ap(lambda a: a.unsqueeze(2).broadcast_to([128, NT, 32])), ALU.mult)
        S.tt(m2[:, :, :], m2[:, :, :], top2[:, :].with_ap(lambda a: a.unsqueeze(2).broadcast_to([128, NT, 32])), ALU.mult)
        S.tt(Wt[:, :, :], m1[:, :, :], m2[:, :, :], ALU.add)
        tap("Wt", Wt[:, :, :], [128, NT, 32], F32)
        tap("Lg", Lg[:, :, :], [128, NT, 36], F32)
        if stop == "route":
            S.emit()
            return nc

        wg_v = wg_d.rearrange("e (kc p) f -> e p kc f", p=128)
        wu_v = wu_d.rearrange("e (kc p) f -> e p kc f", p=128)
        wd_v = wd_d.rearrange("e (fc p) n -> e p fc n", p=128)

        def load_expert(e):
            S.dma(wgu[e % 2][:, 0, :, :], wg_v[e], eng="pool")
            S.dma(wgu[e % 2][:, 1, :, :], wu_v[e], eng="pool")
            S.dma(wdn[e % 3][:, :, :], wd_v[e], eng="pool")

        NE = 32
        load_expert(0)
        n = 0
        ny = 0

        def down_units(e):
            A_ = AT[e % 2]
            wd_ = wdn[e % 3]
            units = []
            for t in range(NT):
                for dh in range(2):
                    units.append((e, A_, wd_, t, dh))
            return units

        def emit_down(unit):
            nonlocal ny
            e, A_, wd_, t, dh = unit
            py = PS[4 + ny % 4]
            ny += 1
            for fc in range(2):
                S.mm(py[:, :], A_[:, fc, t * 128:(t + 1) * 128], wd_[:, fc, dh * 512:(dh + 1) * 512],
                     start=(fc == 0), stop=(fc == 1))
            xv = x1[:, t, dh * 512:(dh + 1) * 512]
            S.stt(xv, py[:, :], Wt[:, t, e:e + 1], xv, ALU.mult, ALU.add)

        pending = []
        for e in range(NE):
            if e + 1 < NE:
                load_expert(e + 1)
            w = wgu[e % 2]
            A = AT[e % 2]
            for tb in range(4):
                ts_ = slice(tb * 512, (tb + 1) * 512)
                for fc in range(2):
                    pg = PS[0 + n % 2]
                    pu = PS[2 + n % 2]
                    n += 1
                    for kc in range(8):
                        S.mm(pg[:, :], w[:, 0, kc, fc * 128:(fc + 1) * 128], h2T[:, kc, ts_], start=(kc == 0), stop=(kc == 7))
                        if kc in (3, 7) and pending:
                            emit_down(pending.pop(0))
                    s_ = sGs[n % 2]
                    S.act(s_[:, :], pg[:, :], AF.Silu)
                    for kc in range(8):
                        S.mm(pu[:, :], w[:, 1, kc, fc * 128:(fc + 1) * 128], h2T[:, kc, ts_], start=(kc == 0), stop=(kc == 7))
                        if kc in (3, 7) and pending:
                            emit_down(pending.pop(0))
                    u_ = uSs[n % 2]
                    S.act(u_[:, :], pu[:, :], AF.Copy)
                    S.tt(A[:, fc, ts_], u_[:, :], s_[:, :], ALU.mult, eng="pool")
            while pending:
                emit_down(pending.pop(0))
            pending = down_units(e)
        RF = Region(R_OB.base, R_OB.size)
        gbc3 = RF.take("gbc3", [DM], F32)
        ot = [RF.take("ot%d" % i, [DM], F32) for i in range(2)]
        jk = RF.take("jk", [DM], BF16)
        S.dma(gbc3[:, :], gfin_d.partition_broadcast(128))
        while pending:
            unit = pending.pop(0)
            emit_down(unit)
            t, dh = unit[3], unit[4]
            if dh == 1:
                S.act(jk[:, :], x1[:, t, :], AF.Square, accum=ss2[:, t:t + 1])
                if t % 4 == 3:
                    t4 = t - 3
                    S.ts(rstd2[:, t4:t4 + 4], ss2[:, t4:t4 + 4], 1.0 / DM, 1e-6, ALU.mult, ALU.add)
                    S.act(rstd2[:, t4:t4 + 4], rstd2[:, t4:t4 + 4], AF.Sqrt)
                    S.recip(rstd2[:, t4:t4 + 4], rstd2[:, t4:t4 + 4])
                    for t_ in range(t4, t4 + 4):
                        o_ = ot[t_ % 2]
                        S.stt(o_[:, :], x1[:, t_, :], rstd2[:, t_:t_ + 1], gbc3[:, :], ALU.mult, ALU.mult)
                        S.dma(out_d[t_ * 128:(t_ + 1) * 128, :], o_[:, :], is_out=True)
        S.emit()
    return nc


_W_NAMES = ["norm_mix_g", "w_in", "cmp_pos_k", "cmp_w1_k", "cmp_w2_k", "cmp_pos_v", "cmp_w1_v", "cmp_w2_v", "sinks",
            "w_a", "w_b", "w_o", "norm_ffn_g", "w_group", "b_group", "w_expert", "b_expert",
            "w_gate_e", "w_up_e", "w_down_e"]


def make_in_maps(inputs, n_cores=8):
    cb, cf = make_consts()
    shared = {"constb": cb, "constf": cf}
    for k in _W_NAMES:
        a = np.ascontiguousarray(np.asarray(inputs[k], dtype=np.float32))
        shared[k] = a.reshape(a.shape[1:]) if a.shape[0] == 1 and a.ndim >= 2 else a
    for k in ("norm_mix_g", "sinks", "norm_ffn_g", "b_group", "b_expert"):
        shared[k] = shared[k].reshape(1, -1)
    shared["norm_final_g"] = np.ascontiguousarray(np.asarray(inputs["norm_final_g"], np.float32)).reshape(1, -1)
    x = np.asarray(inputs["x"], dtype=np.float32)
    return [dict(shared, x=np.ascontiguousarray(x[c])) for c in range(n_cores)]


_NC_CACHE = {}


def kernel(**inputs):
    if "nc" not in _NC_CACHE:
        _NC_CACHE["nc"] = build_program()
    nc = _NC_CACHE["nc"]
    in_maps = make_in_maps(inputs, 8)
    res = run_bass_kernel_spmd(nc, in_maps, core_ids=list(range(8)))
    out = np.stack([np.asarray(r["out"], dtype=np.float32) for r in res.results], axis=0)
    return out
```

```python
import contextlib
import ml_dtypes
from concourse.bass_utils import run_bass_kernel_spmd
import numpy as np
import concourse.bass as bass
import concourse.mybir as mybir

F32 = mybir.dt.float32
BF16 = mybir.dt.bfloat16
I32 = mybir.dt.int32
ALU = mybir.AluOpType
AF = mybir.ActivationFunctionType
AX = mybir.AxisListType

_DT_SIZE = {F32: 4, BF16: 2, I32: 4}


class View:
    __slots__ = ("buf", "ap", "box", "boxes")

    def __init__(self, buf, ap, box, boxes=None):
        self.buf = buf
        self.ap = ap
        self.box = box
        self.boxes = boxes if boxes is not None else [box]

    def with_ap(self, fn):
        return View(self.buf, fn(self.ap), self.box, self.boxes)


class Buf:
    def __init__(self, handle, shape, dtype, name, base_byte=0, alias=None):
        self.h = handle
        self.shape = tuple(shape)
        self.dtype = dtype
        self.name = name
        self.esz = _DT_SIZE[dtype]
        self.base_byte = base_byte
        self.alias = alias if alias is not None else self
        self.psum = False
        st = []
        s = 1
        for d in reversed(self.shape[1:]):
            st.append(s)
            s *= d
        self.fstrides = list(reversed(st))
        self.wr = []
        self.rd = {}
        self.rd_floor = {}
        self.wr_floor = 0

    def __getitem__(self, key):
        if not isinstance(key, tuple):
            key = (key,)
        key = list(key) + [slice(None)] * (len(self.shape) - len(key))
        k0 = key[0]
        if isinstance(k0, slice):
            p0, p1, _ = k0.indices(self.shape[0])
        else:
            p0, p1 = k0, k0 + 1
            key[0] = slice(k0, k0 + 1)
        lo = 0
        hi = 0
        for d, k in enumerate(key[1:]):
            n = self.shape[d + 1]
            if isinstance(k, slice):
                a, b, step = k.indices(n)
                cnt = len(range(a, b, step))
                last = a + (cnt - 1) * step
            else:
                a = k
                last = k
            lo += a * self.fstrides[d]
            hi += last * self.fstrides[d]
        box = (p0, p1, self.base_byte + lo * self.esz, self.base_byte + (hi + 1) * self.esz)
        if self.alias.psum:
            box = (0, 128, 0, 2048)
        return View(self.alias, self.h[tuple(key)], box)


def _overlap(a, b):
    return a[0] < b[1] and b[0] < a[1] and a[2] < b[3] and b[2] < a[3]


def _contains(outer, inner):
    return outer[0] <= inner[0] and outer[1] >= inner[1] and outer[2] <= inner[2] and outer[3] >= inner[3]


class Sched:
    ENGS = ("sync", "act", "dve", "pool", "pe")
    NSLOT = 6

    def __init__(self, nc):
        self.nc = nc
        self.ops = []
        self.eng_ops = {e: [] for e in self.ENGS}
        self.dma_count = {e: 0 for e in self.ENGS}
        self.dma_ops = {e: [] for e in self.ENGS}
        self.out_dmas = []
        self.debug = False

    MAXREC = 12

    def _is_inorder(self, oid):
        o = self.ops[oid]
        return not o["dma"]

    def op(self, eng, fn, reads=(), writes=(), dma=False, is_out=False):
        oid = len(self.ops)
        deps = set()
        reads = [(v.buf, bx) for v in reads for bx in v.boxes]
        writes = [(v.buf, bx) for v in writes for bx in v.boxes]
        for (vb, vbox) in reads:
            for (box, o) in vb.wr:
                if _overlap(box, vbox):
                    deps.add(o)
        for (vb, vbox) in writes:
            for (box, o) in vb.wr:
                if _overlap(box, vbox):
                    deps.add(o)
            for key, lst in vb.rd.items():
                if key == eng and not dma and eng == "pe":
                    continue
                for (box, o) in lst:
                    if _overlap(box, vbox):
                        deps.add(o)
        rec = dict(eng=eng, fn=fn, deps=deps, dma=dma, id=oid)
        if self.debug:
            import sys as _sys
            f = _sys._getframe(1)
            w = []
            while f is not None and len(w) < 4:
                if f.f_code.co_name not in ("op", "dma", "mm", "act", "tt", "ts", "stt", "copy", "memset", "recip", "reduce", "max8", "transpose"):
                    w.append("%s:%d" % (f.f_code.co_name, f.f_lineno))
                f = f.f_back
            rec["where"] = " < ".join(w)
        self.ops.append(rec)
        for (b, vbox) in writes:
            b.wr = [r for r in b.wr if not _contains(vbox, r[0])]
            for key in list(b.rd.keys()):
                b.rd[key] = [r for r in b.rd[key] if not _contains(vbox, r[0])]
            b.wr.append((vbox, oid))
            if len(b.wr) > 4 * self.MAXREC and len(b.wr) > 2 * b.wr_floor:
                self._merge_writes(b)
                b.wr_floor = len(b.wr)
        for (b, vbox) in reads:
            key = ("dma", oid) if dma else eng
            lst = b.rd.setdefault(key, [])
            lst[:] = [r for r in lst if r[0] != vbox]
            lst.append((vbox, oid))
            if len(lst) > self.MAXREC and len(lst) > 2 * b.rd_floor.get(key, 0):
                lst[:] = self._cluster(lst)
                b.rd_floor[key] = len(lst)
        if dma:
            i = self.dma_count[eng]
            self.dma_count[eng] += 1
            rec["dma_i"] = i
            if i >= self.NSLOT:
                deps.add(self.dma_ops[eng][i - self.NSLOT])
            self.dma_ops[eng].append(oid)
            if is_out:
                self.out_dmas.append(oid)
        rec["seq"] = len(self.eng_ops[eng])
        self.eng_ops[eng].append(oid)
        return oid

    @staticmethod
    def _cluster(lst, gap=512):
        lst = sorted(lst, key=lambda r: (r[0][0], r[0][1], r[0][2]))
        out = []
        for (box, o) in lst:
            if out:
                (pb, po) = out[-1]
                if pb[0] == box[0] and pb[1] == box[1] and box[2] <= pb[3] + gap:
                    out[-1] = ((pb[0], pb[1], pb[2], max(pb[3], box[3])), max(po, o))
                    continue
            out.append((box, o))
        return out

    def _merge_writes(self, b):
        groups = {}
        keep = []
        for (box, o) in b.wr:
            op_ = self.ops[o]
            if op_["dma"]:
                keep.append((box, o))
            else:
                groups.setdefault(op_["eng"], []).append((box, o))
        for e, lst in groups.items():
            if len(lst) <= 4:
                keep.extend(lst)
                continue
            keep.extend(self._cluster(lst))
        b.wr = keep

    def emit(self):
        nc = self.nc
        ops = self.ops
        fin = dict(eng="sync", fn=None, deps=set(self.out_dmas), dma=False, id=len(ops), seq=len(self.eng_ops["sync"]))
        self.eng_ops["sync"].append(fin["id"])
        ops.append(fin)
        raw_same = {}
        needed = [False] * len(ops)
        for o in ops:
            for d in o["deps"]:
                dop = ops[d]
                if dop["dma"]:
                    continue
                if dop["eng"] == o["eng"] and not o["dma"]:
                    if o["eng"] == "pe" or o["eng"] == "sync":
                        continue
                needed[d] = True
        cv = {}
        for e in self.ENGS:
            c = 0
            for oid in self.eng_ops[e]:
                o = ops[oid]
                if o["dma"] or o["fn"] is None:
                    continue
                if needed[oid]:
                    c += 1
                    cv[oid] = c
        import contextlib
        with contextlib.ExitStack() as st:
            sem_eng = {e: st.enter_context(nc.semaphore("s_" + e)) for e in ("act", "dve", "pool", "pe")}
            sem_dma = {e: [st.enter_context(nc.semaphore("d_%s%d" % (e, i))) for i in range(self.NSLOT)]
                       for e in ("sync", "act", "pool") if self.dma_count[e] > 0}
            block = st.enter_context(nc.Block())
            sched = self

            def run_engine(ename, e):
                seen = {x: 0 for x in ("act", "dve", "pool", "pe")}
                seen_dma = {}
                for oid in sched.eng_ops[ename]:
                    o = ops[oid]
                    for d in sorted(o["deps"]):
                        dop = ops[d]
                        if dop["dma"]:
                            q = dop["eng"]
                            i = dop["dma_i"]
                            slot = i % sched.NSLOT
                            val = 16 * (i // sched.NSLOT + 1)
                            if seen_dma.get((q, slot), 0) >= val:
                                continue
                            e.wait_ge(sem_dma[q][slot], val)
                            seen_dma[(q, slot)] = val
                        else:
                            de = dop["eng"]
                            if de == ename and not o["dma"] and ename in ("pe", "sync"):
                                continue
                            if d not in cv:
                                continue
                            val = cv[d]
                            if seen[de] >= val:
                                continue
                            e.wait_ge(sem_eng[de], val)
                            seen[de] = val
                    if o["fn"] is None:
                        continue
                    ins = o["fn"](e)
                    if sched.debug:
                        ins.annotate(o["where"])
                    if o["dma"]:
                        i = o["dma_i"]
                        ins.then_inc(sem_dma[ename][i % sched.NSLOT], 16)
                    elif needed[oid]:
                        ins.then_inc(sem_eng[ename], 1)

            @block.sync
            def _(e):
                run_engine("sync", e)

            @block.scalar
            def _(e):
                run_engine("act", e)

            @block.vector
            def _(e):
                run_engine("dve", e)

            @block.gpsimd
            def _(e):
                run_engine("pool", e)

            @block.tensor
            def _(e):
                run_engine("pe", e)

    def dma(self, out, in_, eng="sync", is_out=False, **kw):
        reads = [in_] if isinstance(in_, View) else []
        writes = [out] if isinstance(out, View) else []
        oa = out.ap if isinstance(out, View) else out
        ia = in_.ap if isinstance(in_, View) else in_
        return self.op(eng, lambda e: e.dma_start(out=oa, in_=ia, **kw), reads, writes, dma=True, is_out=is_out)

    def mm(self, out, lhsT, rhs, start=True, stop=True, **kw):
        return self.op("pe", lambda e: e.matmul(out.ap, lhsT.ap, rhs.ap, start=start, stop=stop, **kw),
                       [lhsT, rhs], [out])

    def transpose(self, out, in_, ident):
        return self.op("pe", lambda e: e.transpose(out.ap, in_.ap, ident.ap), [in_, ident], [out])

    def act(self, out, in_, func, bias=None, scale=None, accum=None, eng="act"):
        reads = [in_]
        kw = {}
        if bias is not None:
            if isinstance(bias, View):
                reads.append(bias)
                kw["bias"] = bias.ap
            else:
                kw["bias"] = bias
        if scale is not None:
            if isinstance(scale, View):
                reads.append(scale)
                kw["scale"] = scale.ap
            else:
                kw["scale"] = scale
        writes = [out]
        if accum is not None:
            writes.append(accum)
            kw["accum_out"] = accum.ap
        return self.op(eng, lambda e: e.activation(out.ap, in_.ap, func, **kw), reads, writes)

    def tt(self, out, in0, in1, op, eng="dve"):
        return self.op(eng, lambda e: e.tensor_tensor(out.ap, in0.ap, in1.ap, op), [in0, in1], [out])

    def ts(self, out, in0, s1, s2, op0, op1=None, eng="dve", accum=None):
        reads = [in0]
        a1 = s1
        a2 = s2
        if isinstance(s1, View):
            reads.append(s1)
            a1 = s1.ap
        if isinstance(s2, View):
            reads.append(s2)
            a2 = s2.ap
        writes = [out]
        kw = {}
        if op1 is not None:
            kw["op1"] = op1
        if accum is not None:
            writes.append(accum)
            kw["accum_out"] = accum.ap
        return self.op(eng, lambda e: e.tensor_scalar(out.ap, in0.ap, a1, a2, op0, **kw), reads, writes)

    def stt(self, out, in0, scalar, in1, op0, op1, eng="dve"):
        reads = [in0, in1]
        sc = scalar
        if isinstance(scalar, View):
            reads.append(scalar)
            sc = scalar.ap
        return self.op(eng, lambda e: e.scalar_tensor_tensor(out.ap, in0.ap, sc, in1.ap, op0, op1), reads, [out])

    def copy(self, out, in_, eng="dve"):
        if eng == "act":
            return self.op(eng, lambda e: e.copy(out.ap, in_.ap), [in_], [out])
        return self.op(eng, lambda e: e.tensor_copy(out.ap, in_.ap), [in_], [out])

    def memset(self, out, val, eng="pool"):
        return self.op(eng, lambda e: e.memset(out.ap, val), [], [out])

    def recip(self, out, in_):
        return self.op("dve", lambda e: e.reciprocal(out.ap, in_.ap), [in_], [out])

    def reduce(self, out, in_, op, axis=AX.X, eng="dve"):
        return self.op(eng, lambda e: e.tensor_reduce(out.ap, in_.ap, axis, op), [in_], [out])

    def max8(self, out, in_):
        return self.op("dve", lambda e: e.max(out.ap, in_.ap), [in_], [out])


SEQ = 2048
DM = 1024
NT = 16
NEG = -30000.0
BF = ml_dtypes.bfloat16


def _slopes():
    n = 16
    s = 2.0 ** (-8.0 * np.arange(1, n + 1) / n)
    return s[:8].astype(np.float64), s[8:].astype(np.float64)


CB = {}
CF = {}


def _layout():
    o = 0
    for name, n in (("identb", 128), ("esel", SEQ), ("cmpmask", SEQ), ("ovp", 33), ("tri", 128),
                    ("band_win", 8 * 3 * 128), ("band_swa", 8 * 2 * 128)):
        CB[name] = (o, n)
        o += n
    CB["_n"] = o + (o % 2)
    o = 0
    for name, n in (("identf", 128), ("forced", NT * 32), ("bias_cmp", 32), ("bias_slc", 8 * 19),
                    ("bias_band", 16), ("sq_swa", 8)):
        CF[name] = (o, n)
        o += n
    CF["_n"] = o


_layout()


def make_consts():
    s_swa, s_nsa = _slopes()
    cb = np.zeros((128, CB["_n"]), np.float32)
    cf = np.zeros((128, CF["_n"]), np.float32)

    def putb(name, arr):
        c0, n = CB[name]
        arr = np.asarray(arr, np.float32).reshape(arr.shape[0], -1)
        assert arr.shape[1] == n, (name, arr.shape, n)
        cb[:arr.shape[0], c0:c0 + n] = arr

    def putf(name, arr):
        c0, n = CF[name]
        arr = np.asarray(arr, np.float32).reshape(arr.shape[0], -1)
        assert arr.shape[1] == n, (name, arr.shape, n)
        cf[:arr.shape[0], c0:c0 + n] = arr

    putb("identb", np.eye(128))
    putf("identf", np.eye(128))
    k = np.arange(SEQ)
    putb("esel", (k[None, :] // 64 == np.arange(32)[:, None]).astype(np.float32))
    c = np.arange(127)
    endc = 16 * c + 31
    putb("cmpmask", np.where(endc[:, None] <= k[None, :], 0.0, NEG))
    c0 = c * 16
    s0 = np.arange(32) * 64
    ov = np.clip(np.minimum(c0[:, None] + 32, s0[None, :] + 64) - np.maximum(c0[:, None], s0[None, :]), 0, None) / 32.0
    putb("ovp", np.concatenate([ov, np.ones((127, 1))], axis=1))
    kk = np.arange(128)
    putb("tri", (kk[:, None] <= kk[None, :]).astype(np.float32))
    def band_f(slopes, nwin, window):
        f = np.zeros((128, 8, nwin, 128))
        for h in range(8):
            for mi in range(nwin):
                m = nwin - 1 - mi
                dist = 128 * m + kk[None, :] - kk[:, None]
                e_ = -slopes[h] * dist - slopes[h] * (kk[:, None] - 127) / 2.0
                f[:, h, mi, :] = np.where((dist >= 0) & (dist < window), np.exp(np.minimum(e_, 80.0)), 0.0)
        return f
    putb("band_win", band_f(s_nsa, 3, 256))
    putb("band_swa", band_f(s_swa, 2, 128))
    tq = (np.arange(NT)[None, :] * 128 + kk[:, None])
    cur = tq // 64
    j = np.arange(32)[None, None, :]
    forced = ((j == 0) | (j == cur[:, :, None]) | (j == cur[:, :, None] - 1))
    putf("forced", np.where(forced, 1e9, 0.0))
    bc = np.zeros((128, 8, 4))
    for h in range(8):
        for Q in range(4):
            bc[:127, h, Q] = s_nsa[h] * (endc - (512 * Q + 256))
    putf("bias_cmp", bc)
    bsl = np.zeros((128, 8, 19))
    for h in range(8):
        for dk in range(-15, 4):
            bsl[:, h, dk + 15] = s_nsa[h] * (128 * dk + kk - 256)
    putf("bias_slc", bsl)
    bb = np.zeros((128, 16))
    for h in range(8):
        bb[:, h] = s_nsa[h] * (kk - 127) / 2.0
        bb[:, 8 + h] = s_swa[h] * (kk - 127) / 2.0
    putf("bias_band", bb)
    putf("sq_swa", np.zeros((128, 8)))
    return cb.astype(BF), cf.astype(np.float32)


C_QA, C_KCMP, C_VCMP, C_KSLC, C_VSLC, C_KWIN, C_VWIN, C_GNSA, C_QB, C_KB, C_VB, C_GM = (
    0, 512, 640, 768, 896, 1024, 1152, 1280, 1304, 1816, 1944, 2072)


def build_program(dbg=None, stop=None):
    dbg = dbg or []
    nc = bass.Bass("TRN2", target_bir_lowering=False)

    def din(name, shape, dt=F32):
        return nc.dram_tensor(name, list(shape), dt, kind="ExternalInput").ap()

    x_d = din("x", [SEQ, DM])
    constb_d = din("constb", [128, CB["_n"]], BF16)
    constf_d = din("constf", [128, CF["_n"]])
    gmix_d = din("norm_mix_g", [1, DM])
    win_d = din("w_in", [DM, 4120])
    posk_d = din("cmp_pos_k", [32, 64])
    w1k_d = din("cmp_w1_k", [2048, 256])
    w2k_d = din("cmp_w2_k", [256, 64])
    posv_d = din("cmp_pos_v", [32, 64])
    w1v_d = din("cmp_w1_v", [2048, 256])
    w2v_d = din("cmp_w2_v", [256, 64])
    sinks_d = din("sinks", [1, 8])
    wa_d = din("w_a", [512, DM])
    wb_d = din("w_b", [512, DM])
    wo_d = din("w_o", [DM, DM])
    gffn_d = din("norm_ffn_g", [1, DM])
    wgrp_d = din("w_group", [DM, 4])
    bgrp_d = din("b_group", [1, 4])
    wexp_d = din("w_expert", [DM, 32])
    bexp_d = din("b_expert", [1, 32])
    wg_d = din("w_gate_e", [32, DM, 256])
    wu_d = din("w_up_e", [32, DM, 256])
    wd_d = din("w_down_e", [32, 256, DM])
    gfin_d = din("norm_final_g", [1, DM])
    out_d = nc.dram_tensor("out", [SEQ, DM], F32, kind="ExternalOutput").ap()

    ARENA_BYTES = 212000
    with contextlib.ExitStack() as st:
        arena_h = st.enter_context(nc.sbuf_tensor("arena", [128, ARENA_BYTES // 2], BF16))
        ps_h = [st.enter_context(nc.psum_tensor("ps%d" % i, [128, 512], F32)) for i in range(8)]
        S = Sched(nc)
        S.debug = bool(globals().get('DEBUG_SCHED', False))
        ARENA = Buf(arena_h, [128, ARENA_BYTES // 2], BF16, "arena")

        def carve(name, fshape, dtype, at):
            fshape = list(fshape)
            n = int(np.prod(fshape))
            nb = n * _DT_SIZE[dtype]
            assert at % 4 == 0 and at + nb <= ARENA_BYTES, (name, at, nb)
            ap = arena_h[:, at // 2:(at + nb) // 2]
            if dtype == F32:
                ap = ap.bitcast(F32)
            if len(fshape) == 2:
                ap = ap.rearrange("p (a b) -> p a b", a=fshape[0])
            elif len(fshape) == 3:
                ap = ap.rearrange("p (a b c) -> p a b c", a=fshape[0], b=fshape[1])
            elif len(fshape) == 4:
                ap = ap.rearrange("p (a b c d) -> p a b c d", a=fshape[0], b=fshape[1], c=fshape[2])
            b = Buf(ap, [128] + fshape, dtype, name, base_byte=at, alias=ARENA)
            b.nbytes = nb
            return b

        class Region:
            def __init__(self, base, size):
                self.base, self.size, self.off = base, size, 0

            def take(self, name, fshape, dtype):
                self.off = (self.off + 3) // 4 * 4
                b = carve(name, fshape, dtype, self.base + self.off)
                self.off += b.nbytes
                assert self.off <= self.size, (name, self.off, self.size)
                return b

        PS = [Buf(p, [128, 512], F32, "ps%d" % i) for i, p in enumerate(ps_h)]
        for p_ in PS:
            p_.psum = True
        PSB = [Buf(p.bitcast(BF16), [128, 1024], BF16, "psb%d" % i, alias=PS[i]) for i, p in enumerate(ps_h)]

        def r3(view, pat, **kw):
            return view.with_ap(lambda a: a.rearrange(pat, **kw))

        _rb = [0]

        def _reg(size):
            r = Region(_rb[0], size)
            _rb[0] += size
            return r

        R_CONST = _reg(30720)
        R_HT = _reg(32768)
        R_Q = _reg(32768)
        R_K = _reg(32768)
        R_V = _reg(12544)
        R_SEL = _reg(8192)
        R_PT = _reg(12288)
        R_OA = _reg(32768)
        R_OB = _reg(16384)
        assert _rb[0] <= ARENA_BYTES, _rb[0]

        constb = R_CONST.take("constb", [CB["_n"]], BF16)
        constf = R_CONST.take("constf", [CF["_n"]], F32)
        gnsa = R_CONST.take("gnsa", [NT, 24], F32)
        ssq = R_CONST.take("ssq", [NT], F32)
        rstd = R_CONST.take("rstd", [NT], F32)
        kcT = R_CONST.take("kcT", [2, 128], BF16)
        vcp = R_CONST.take("vcp", [2, 65], BF16)
        sinkt = R_CONST.take("sinkt", [8], F32)
        smallf = R_CONST.take("smallf", [64], F32)

        def cbv(name, p0=0, p1=128):
            c0, n = CB[name]
            return constb, c0, n

        def CBs(name, rows=slice(0, 128), cols=None):
            c0, n = CB[name]
            if cols is None:
                cols = slice(0, n)
            return constb[rows, c0 + cols.start:c0 + cols.stop]

        def CFs(name, rows=slice(0, 128), cols=None):
            c0, n = CF[name]
            if cols is None:
                cols = slice(0, n)
            return constf[rows, c0 + cols.start:c0 + cols.stop]

        identb = CBs("identb")
        identf = CFs("identf")

        hT = R_HT.take("hT", [8, SEQ], BF16)
        qaT = R_Q.take("qaT", [4, SEQ], BF16)
        qbT = R_Q.take("qbT", [4, SEQ], BF16)
        kcmpT = R_K.take("kcmpT", [SEQ], BF16)
        vcmpT = R_K.take("vcmpT", [SEQ], BF16)
        kslc = R_K.take("kslc", [2, SEQ], BF16)
        kwin = R_K.take("kwin", [2, SEQ], BF16)
        kb = R_K.take("kb", [2, SEQ], BF16)
        vtm = R_V.take("vtm", [NT, 6, 65], BF16)
        selT = R_SEL.take("selT", [2, SEQ], BF16)
        NPT = 6
        PTb = [R_PT.take("pt%d" % i, [512], BF16) for i in range(NPT)]
        evs = R_PT.take("evs", [512], F32)
        oa = R_OA.take("oa", [NT, 8, 64], F32)
        ob = R_OB.take("ob", [NT, 8, 64], BF16)

        debug_outs = []

        def tap(name, buf_view, shape, dt=F32):
            if name not in dbg:
                return
            d = nc.dram_tensor("dbg_" + name, list(shape), dt, kind="ExternalOutput").ap()
            S.dma(d, buf_view, is_out=True)

        S.dma(constb[:, :], constb_d)
        S.dma(constf[:, :], constf_d)

        xall = Buf(arena_h[:, R_Q.base // 2:(R_Q.base + 65536) // 2].bitcast(F32).rearrange("p (t d) -> p t d", t=NT),
                   [128, NT, DM], F32, "xall", base_byte=R_Q.base, alias=ARENA)
        PW = Region(R_OA.base, R_OA.size)
        wst = [PW.take("wst%d" % i, [8, 128], BF16) for i in range(16)]
        P0 = Region(R_OB.base, R_OB.size)
        wtm = P0.take("wtm", [8, 408], BF16)
        gbc = P0.take("gbc", [DM], F32)
        hb = [P0.take("hb%d" % i, [DM], BF16) for i in range(2)]
        PJ = Region(R_SEL.base, R_SEL.size + R_PT.size)
        w1k_buf = PJ.take("w1k", [32, 256], BF16)
        junk = PJ.take("junk", [DM], BF16)

        win_v = win_d.rearrange("(kc p) n -> p kc n", p=128)
        S.dma(gbc[:, :], gmix_d.partition_broadcast(128))

        def xtile(t):
            a_, b_ = t // 4, t % 4
            lo = (4 * b_) * 4096 + a_ * 1024
            hi = (4 * b_ + 3) * 4096 + a_ * 1024 + 1024
            ap = xall.h.rearrange("p s (q c) -> p s q c", q=4)[:, 4 * b_:4 * b_ + 4, a_, :]
            boxes = [(0, 128, R_Q.base + (4 * b_ + j) * 4096 + a_ * 1024, R_Q.base + (4 * b_ + j) * 4096 + a_ * 1024 + 1024)
                     for j in range(4)]
            return View(ARENA, ap, (0, 128, R_Q.base + lo, R_Q.base + hi), boxes)

        for t in range(NT):
            S.dma(xtile(t), x_d[t * 128:(t + 1) * 128, :].rearrange("p (j c) -> p j c", j=4))
        fm = []
        for c in range(4):
            fm.append((lambda tb, c=c: qaT[:, c, tb * 512:(tb + 1) * 512], [(C_QA + c * 128, 128)], 0.125))
        fm.append((lambda tb: kcmpT[:, tb * 512:(tb + 1) * 512], [(C_KCMP, 128)], 1.0))
        fm.append((lambda tb: vcmpT[:, tb * 512:(tb + 1) * 512], [(C_VCMP, 128)], 1.0))
        for (dst, c0) in ((kslc, C_KSLC), (kwin, C_KWIN)):
            for g in range(2):
                fm.append((lambda tb, dst=dst, g=g: dst[:, g, tb * 512:(tb + 1) * 512],
                           [(c0 + g * 64, 64), (c0 + g * 64, 64)], 1.0))
        for c in range(4):
            fm.append((lambda tb, c=c: qbT[:, c, tb * 512:(tb + 1) * 512], [(C_QB + c * 128, 128)], 0.125))
        for g in range(2):
            fm.append((lambda tb, g=g: kb[:, g, tb * 512:(tb + 1) * 512],
                       [(C_KB + g * 64, 64), (C_KB + g * 64, 64)], 1.0))
        for ci, (dstf, pieces, scale) in enumerate(fm):
            o = 0
            for (c0, n) in pieces:
                S.dma(wst[ci][:, :, o:o + n], win_v[:, :, c0:c0 + n], eng="pool")
                o += n
        for i, c0 in enumerate((C_VSLC, C_VWIN, C_VB)):
            S.dma(wtm[:, :, i * 128:(i + 1) * 128], win_v[:, :, c0:c0 + 128], eng="pool")
        S.dma(wtm[:, :, 384:408], win_v[:, :, C_GNSA:C_GNSA + 24], eng="pool")
        w1kv_ = w1k_d.rearrange("(l d) h -> d l h", d=64)
        S.dma(w1k_buf[0:64, :, :], w1kv_, eng="pool")
        S.dma(w1k_buf[64:128, :, :], w1kv_, eng="pool")

        def p0_group(t4):
            j3 = r3(junk[:, :], "p (j c) -> p j c", j=4)
            for t in range(t4, t4 + 4):
                S.act(j3, xtile(t), AF.Square, accum=ssq[:, t:t + 1])
            S.ts(rstd[:, t4:t4 + 4], ssq[:, t4:t4 + 4], 1.0 / DM, 1e-6, ALU.mult, ALU.add)
            S.act(rstd[:, t4:t4 + 4], rstd[:, t4:t4 + 4], AF.Sqrt)
            S.recip(rstd[:, t4:t4 + 4], rstd[:, t4:t4 + 4])
            for t in range(t4, t4 + 4):
                h = hb[t % 2]
                S.stt(r3(h[:, :], "p (j c) -> p j c", j=4), xtile(t), rstd[:, t:t + 1],
                      r3(gbc[:, :], "p (j c) -> p j c", j=4), ALU.mult, ALU.mult)
                pb = PSB[t % 2]
                for c in range(8):
                    S.transpose(pb[:, c * 128:(c + 1) * 128], h[:, c * 128:(c + 1) * 128], identb)
                S.copy(hT[:, :, t * 128:(t + 1) * 128], r3(pb[:, 0:1024], "p (c n) -> p c n", c=8),
                       eng=("act" if t % 2 else "dve"))

        pj = {"nev": 0}

        def proj_block(tb):
            for ci, (dstf, pieces, scale) in enumerate(fm):
                w = wst[ci]
                p = PS[2 + (pj["nev"] % 4)]
                for kc in range(8):
                    S.mm(p[:, :], w[:, kc, :], hT[:, kc, tb * 512:(tb + 1) * 512], start=(kc == 0), stop=(kc == 7))
                if pj["nev"] % 2 == 0:
                    S.act(dstf(tb), p[:, :], AF.Copy, scale=scale)
                else:
                    S.ts(dstf(tb), p[:, :], scale, None, ALU.mult)
                pj["nev"] += 1
            for t in range(4 * tb, 4 * tb + 4):
                p = PS[6 + t % 2]
                for kc in range(8):
                    S.mm(p[:, 0:408], hT[:, kc, t * 128:(t + 1) * 128], wtm[:, kc, :], start=(kc == 0), stop=(kc == 7))
                S.copy(vtm[:, t, :, 0:64], r3(p[:, 0:384], "p (s d) -> p s d", s=6), eng="dve")
                S.act(gnsa[:, t, :], p[:, 384:408], AF.Sigmoid)

        S.memset(vtm[:, :, :, 64:65], 1.0)
        p0_group(0)
        p0_group(4)
        proj_block(0)
        p0_group(8)
        proj_block(1)
        p0_group(12)
        proj_block(2)
        proj_block(3)
        tap("hT", hT[:, :, :], [128, 8, SEQ], BF16)
        if stop == "p0":
            S.emit()
            return nc

        for nm, b, sh in (("qaT", qaT, [128, 4, SEQ]), ("qbT", qbT, [128, 4, SEQ]), ("kslc", kslc, [128, 2, SEQ]),
                          ("kcmpT", kcmpT, [128, SEQ]), ("vtm", vtm, [128, NT, 6, 65])):
            tap(nm, b[(slice(None),) * len(sh)], sh, BF16)
        tap("gnsa", gnsa[:, :, :], [128, NT, 24], F32)
        if stop == "proj":
            S.emit()
            return nc

        PC = Region(R_OA.base, R_OA.size + R_OB.size)
        w1d = {"k": w1k_buf, "v": PC.take("w1v", [32, 256], BF16)}
        w2kd = PC.take("w2kd", [2, 128], BF16)
        w2v = PC.take("w2v", [2, 64], BF16)
        posT = {"k": PC.take("posTk", [32], BF16), "v": PC.take("posTv", [32], BF16)}
        posb = {"k": PC.take("posbk", [2], F32), "v": PC.take("posbv", [2], F32)}
        hidT = {"k": PC.take("hidTk", [2, 2, 128], BF16), "v": PC.take("hidTv", [2, 2, 128], BF16)}
        for X, w1_d, pos_d in (("k", w1k_d, posk_d), ("v", w1v_d, posv_d)):
            if X == "v":
                w1v_ = w1_d.rearrange("(l d) h -> d l h", d=64)
                S.dma(w1d[X][0:64, :, :], w1v_, eng="pool")
                S.dma(w1d[X][64:128, :, :], w1v_, eng="pool")
            S.dma(posT[X][0:64, :], pos_d.rearrange("l d -> d l"), eng="pool", allow_slow_non_contiguous=True)
        w2k_v = w2k_d.rearrange("(hc p) d -> p hc d", p=128)
        S.dma(w2kd[:, :, 0:64], w2k_v, eng="pool")
        S.dma(w2kd[:, :, 64:128], w2k_v, eng="pool")
        S.dma(w2v[:, :, :], w2v_d.rearrange("(hc p) d -> p hc d", p=128), eng="pool")
        S.memset(vcp[:, :, 64:65], 1.0)
        for X, srcT in (("k", kcmpT), ("v", vcmpT)):
            pp = PS[0]
            for hc in range(2):
                for l in range(32):
                    S.mm(pp[:, hc:hc + 1], w1d[X][0:64, l, hc * 128:(hc + 1) * 128], posT[X][0:64, l:l + 1],
                         start=(l == 0), stop=(l == 31))
            S.copy(posb[X][:, :], pp[:, 0:2])
            for hc in range(2):
                pg_ = [PS[1 + 2 * hc], PS[2 + 2 * hc]]
                for l in range(32):
                    for g in range(2):
                        S.mm(pg_[g][:, 0:127], w1d[X][64 * g:64 * g + 64, l, hc * 128:(hc + 1) * 128],
                             srcT[64 * g:64 * g + 64, l:l + 16 * 126 + 1:16], start=(l == 0), stop=(l == 31))
                for g in range(2):
                    S.act(hidT[X][:, g, hc, 0:127], pg_[g][:, 0:127], AF.Silu, bias=posb[X][:, hc:hc + 1])
        for g in range(2):
            p = PS[5 + g]
            for hc in range(2):
                S.mm(p[:, g * 128:g * 128 + 127], w2kd[:, hc, :], hidT["k"][:, g, hc, 0:127], start=(hc == 0), stop=(hc == 1))
            S.copy(kcT[:, g, 0:127], p[:, g * 128:g * 128 + 127])
        for g in range(2):
            p = PS[6 + g]
            for hc in range(2):
                S.mm(p[0:127, g * 64:(g + 1) * 64], hidT["v"][:, g, hc, 0:127], w2v[:, hc, :], start=(hc == 0), stop=(hc == 1))
            S.copy(vcp[0:127, g, 0:64], p[0:127, g * 64:(g + 1) * 64], eng="act")
        RG0 = Region(R_K.base, 8192)
        wgs = [RG0.take("wgs%d" % i, [8, 256], BF16) for i in range(2)]
        wgs += [None, None]
        gpieces = {}

        def load_gate_piece(fp):
            for ab in range(2):
                w = wgs[(2 * fp + ab) % 4]
                c0 = C_GM + ab * 1024 + fp * 256
                S.dma(w[:, :, :], win_v[:, :, c0:c0 + 256], eng="pool")
                gpieces[(fp, ab)] = w

        load_gate_piece(0)
        tap("kcT", kcT[:, :, 0:127], [128, 2, 127], BF16)
        tap("vcp", vcp[0:127, :, :], [127, 2, 65], BF16)
        if stop == "cmpmlp":
            S.emit()
            return nc

        sk = smallf[:, 0:8]
        S.dma(sk, sinks_d.partition_broadcast(128))
        S.tt(sk, sk, CFs("sq_swa"), ALU.add)
        S.act(sinkt[:, :], sk, AF.Exp)

        imp_sb = R_PT.take("imp_sb", [4, 2, 32], F32)
        selb = R_PT.take("selb", [4, 2, 32], BF16)
        Msb = [R_PT.take("msb%d" % i, [512], BF16) for i in range(2)]
        top8 = R_PT.take("top8", [8], F32)
        ev_rz = evs[:, 0:4]
        ev_coef = evs[:, 4:8]
        ev_rzi = evs[:, 8:12]
        ev_tmp = evs[:, 64:64 + 256]
        ev_tmp2 = evs[:, 320:320 + 128]
        S.memset(selT[:, :, :], 0.0)

        st_banks = [0, 1, 2, 3]
        cnt = {"st": 0, "pt": 0}

        def next_st():
            b_ = PS[st_banks[cnt["st"] % 4]]
            cnt["st"] += 1
            return b_

        def next_pt():
            b_ = PTb[cnt["pt"] % NPT]
            cnt["pt"] += 1
            return b_

        def accv(p, u0, u1, d0, d1):
            v = p[:, u0 * 65:(u1 - 1) * 65 + d1]
            ap = p.h[:, 0:260].rearrange("p (u d) -> p u d", u=4)[:, u0:u1, d0:d1]
            return View(v.buf, ap, v.box)

        def acc1(p, u):
            return accv(p, u, u + 1, 0, 65).with_ap(lambda a: a[:, 0, :])

        def evac_nsa(p, Q, h, br, first):
            z = accv(p, 0, 4, 64, 65)
            S.ts(ev_rz.with_ap(lambda a: a.unsqueeze(2)), z, 1e-30, None, ALU.max)
            S.recip(ev_rz, ev_rz)
            gate = gnsa[:, 4 * Q:4 * Q + 4, h * 3 + br]
            S.tt(ev_coef, ev_rz, gate, ALU.mult)
            cb_ = ev_coef.with_ap(lambda a: a.unsqueeze(2).broadcast_to([128, 4, 64]))
            o = oa[:, 4 * Q:4 * Q + 4, h, :]
            if first:
                S.tt(o, accv(p, 0, 4, 0, 64), cb_, ALU.mult)
            else:
                t3 = r3(ev_tmp, "p (u d) -> p u d", u=4)
                S.tt(t3, accv(p, 0, 4, 0, 64), cb_, ALU.mult)
                S.tt(o, o, t3, ALU.add, eng="pool")

        HALF = ((0, 64), (64, 128))

        def do_cmp(Q):
            qs = slice(Q * 512, (Q + 1) * 512)
            pend = []

            def finish(step):
                c, pts = step
                g = c // 2
                hs = (2 * c, 2 * c + 1)
                for e in range(2):
                    accp = PS[4 + e]
                    impp = PS[6 + e]
                    pt = pts[e]
                    for u in range(4):
                        S.mm(acc1(accp, u), pt[0:127, u * 128:(u + 1) * 128], vcp[0:127, g, :],
                             start=(u == 0), stop=True, skip_group_check=True)
                    for u in range(4):
                        S.mm(impp[:, u * 33:(u + 1) * 33], pt[0:127, u * 128:(u + 1) * 128], CBs("ovp", slice(0, 127)),
                             start=(u == 0), stop=True, skip_group_check=True)
                for e in range(2):
                    h = hs[e]
                    accp = PS[4 + e]
                    impp = PS[6 + e]
                    evac_nsa(accp, Q, h, 0, True)
                    zi = View(impp, impp.h[:, 0:132].rearrange("p (u d) -> p u d", u=4)[:, :, 32:33], impp[:, 0:132].box)
                    S.ts(ev_rzi.with_ap(lambda a: a.unsqueeze(2)), zi, 1e-30, None, ALU.max)
                    S.recip(ev_rzi, ev_rzi)
                    rb = ev_rzi.with_ap(lambda a: a.unsqueeze(2).broadcast_to([128, 4, 32]))
                    iv = View(impp, impp.h[:, 0:132].rearrange("p (u d) -> p u d", u=4)[:, :, 0:32], impp[:, 0:132].box)
                    if h % 4 == 0:
                        S.tt(imp_sb[:, :, g, :], iv, rb, ALU.mult)
                    else:
                        t3 = r3(ev_tmp2, "p (u d) -> p u d", u=4)
                        S.tt(t3, iv, rb, ALU.mult)
                        S.tt(imp_sb[:, :, g, :], imp_sb[:, :, g, :], t3, ALU.add, eng="pool")

            for c in range(4):
                g = c // 2
                hs = (2 * c, 2 * c + 1)
                stp = [next_st(), next_st()]
                for e in range(2):
                    lo, hi = HALF[e]
                    S.mm(stp[e][0:127, :], kcT[lo:hi, g, 0:127], qaT[lo:hi, c, qs], start=True, stop=False)
                for e in range(2):
                    S.mm(stp[e][0:127, :], CBs("identb", slice(0, 127), slice(0, 127)), CBs("cmpmask", slice(0, 127), qs),
                         start=False, stop=True)
                pts = [next_pt(), next_pt()]
                for e in range(2):
                    h = hs[e]
                    S.act(pts[e][0:127, :], stp[e][0:127, :], AF.Exp,
                          bias=CFs("bias_cmp", slice(0, 127), slice(h * 4 + Q, h * 4 + Q + 1)))
                pend.append((c, pts))
                if len(pend) > 1:
                    finish(pend.pop(0))
            while pend:
                finish(pend.pop(0))

        def do_select(Q):
            c0, _ = CF["forced"]
            fv = View(constf.alias, constf.h[:, c0 + 4 * Q * 32:c0 + (4 * Q + 4) * 32].rearrange("p (u j) -> p u j", u=4)
                      .unsqueeze(2).broadcast_to([128, 4, 2, 32]), constf[:, c0 + 4 * Q * 32:c0 + (4 * Q + 4) * 32].box)
            S.tt(imp_sb[:, :, :, :], imp_sb[:, :, :, :], fv, ALU.max)
            for g in range(2):
                pb = PSB[6 + g]
                for u in range(4):
                    S.max8(top8[:, :], imp_sb[:, u, g, :])
                    S.ts(selb[:, u, g, :], imp_sb[:, u, g, :], top8[:, 7:8], None, ALU.is_ge)
                    S.transpose(pb[0:32, u * 128:(u + 1) * 128], selb[:, u, g, :], identb)
                S.copy(selT[0:32, g, Q * 512:(Q + 1) * 512], pb[0:32, 0:512], eng="act")

        msb_cnt = [0]

        def do_slc(Q):
            nk = 4 * Q + 4
            for g in range(2):
                cs = (2 * g, 2 * g + 1)
                accs = {cs[0]: [PS[4], PS[5]], cs[1]: [PS[6], PS[7]]}
                pend = []

                def pv(kt, c, pts):
                    r = kt - 4 * Q
                    for e in range(2):
                        for u in range(max(r, 0), 4):
                            S.mm(acc1(accs[c][e], u), pts[e][:, u * 128:(u + 1) * 128],
                                 vtm[:, kt, 0 * 2 + g, :], start=(kt == 0 and u == 0), stop=True, skip_group_check=True)

                def make_mask(kt):
                    r = kt - 4 * Q
                    c0 = 128 * r if r > 0 else 0
                    mp = next_st()
                    S.mm(mp[:, c0:512], CBs("esel", slice(0, 128), slice(kt * 128, (kt + 1) * 128)),
                         selT[:, g, Q * 512 + c0:(Q + 1) * 512], start=True, stop=True)
                    ms = Msb[msb_cnt[0] % 2]
                    msb_cnt[0] += 1
                    S.act(ms[:, c0:512], mp[:, c0:512], AF.Copy)
                    if r >= 0:
                        S.tt(ms[:, c0:c0 + 128], ms[:, c0:c0 + 128], CBs("tri"), ALU.mult, eng="pool")
                    return ms

                ms_next = make_mask(0)
                for kt in range(nk):
                    r = kt - 4 * Q
                    c0 = 128 * r if r > 0 else 0
                    ms = ms_next
                    if kt + 1 < nk:
                        ms_next = make_mask(kt + 1)
                    for c in cs:
                        hs = (2 * c, 2 * c + 1)
                        stp = [next_st(), next_st()]
                        for e in range(2):
                            lo, hi = HALF[e]
                            S.mm(stp[e][:, c0:512], kslc[lo:hi, g, kt * 128:(kt + 1) * 128],
                                 qaT[lo:hi, c, Q * 512 + c0:(Q + 1) * 512], start=True, stop=True)
                        pts = [next_pt(), next_pt()]
                        for e in range(2):
                            h = hs[e]
                            bcol = h * 19 + (kt - 4 * Q + 15)
                            S.act(pts[e][:, c0:512], stp[e][:, c0:512], AF.Exp,
                                  bias=CFs("bias_slc", slice(0, 128), slice(bcol, bcol + 1)))
                            S.tt(pts[e][:, c0:512], pts[e][:, c0:512], ms[:, c0:512], ALU.mult)
                        pend.append((kt, c, pts))
                        if len(pend) > 1:
                            pv(*pend.pop(0))
                while pend:
                    pv(*pend.pop(0))
                for c in cs:
                    for e in range(2):
                        evac_nsa(accs[c][e], Q, 2 * c + e, 1, False)

        band_cnt = [0]

        def do_band(Q, which):
            nwin = 3 if which == "win" else 2
            if which == "win":
                qT_, kT_, vbase, band = qaT, kwin, 1 * 2, "band_win"
            else:
                qT_, kT_, vbase, band = qbT, kb, 2 * 2, "band_swa"
            bc0, _ = CB[band]
            pend = []

            def finish(step):
                c, u, js, pts, accs, i = step
                g = c // 2
                hs = (2 * c, 2 * c + 1)
                for e in range(2):
                    for j in js:
                        mi = j - (i - nwin + 1)
                        S.mm(acc1(accs[e], u), pts[e][:, mi * 128:(mi + 1) * 128],
                             vtm[:, j, vbase + g, :], start=(u == 0 and j == js[0]), stop=True, skip_group_check=True)
                if u == 3:
                    for e in range(2):
                        h = hs[e]
                        if which == "win":
                            evac_nsa(accs[e], Q, h, 2, False)
                        else:
                            z = accv(accs[e], 0, 4, 64, 65)
                            S.ts(ev_rz.with_ap(lambda a: a.unsqueeze(2)), z, sinkt[:, h:h + 1], None, ALU.add)
                            S.recip(ev_rz, ev_rz)
                            cb_ = ev_rz.with_ap(lambda a: a.unsqueeze(2).broadcast_to([128, 4, 64]))
                            S.tt(ob[:, 4 * Q:4 * Q + 4, h, :], accv(accs[e], 0, 4, 0, 64), cb_, ALU.mult)

            for c in range(4):
                g = c // 2
                hs = (2 * c, 2 * c + 1)
                ab = 4 + 2 * (band_cnt[0] % 2)
                band_cnt[0] += 1
                accs = [PS[ab], PS[ab + 1]]
                for u in range(4):
                    i = 4 * Q + u
                    js = [j for j in range(i - nwin + 1, i + 1) if j >= 0]
                    stp = [next_st(), next_st()]
                    for j in js:
                        mi = j - (i - nwin + 1)
                        for e in range(2):
                            lo, hi = HALF[e]
                            S.mm(stp[e][:, mi * 128:(mi + 1) * 128], kT_[lo:hi, g, j * 128:(j + 1) * 128],
                                 qT_[lo:hi, c, i * 128:(i + 1) * 128], start=(j == js[0]), stop=True, skip_group_check=True)
                    mi0 = js[0] - (i - nwin + 1)
                    pts = [next_pt(), next_pt()]
                    for e in range(2):
                        h = hs[e]
                        hcol = h if which == "win" else 8 + h
                        S.act(pts[e][:, mi0 * 128:nwin * 128], stp[e][:, mi0 * 128:nwin * 128], AF.Exp,
                              bias=CFs("bias_band", slice(0, 128), slice(hcol, hcol + 1)))
                        boff = bc0 + (h * nwin + mi0) * 128
                        S.tt(pts[e][:, mi0 * 128:nwin * 128], pts[e][:, mi0 * 128:nwin * 128],
                             constb[:, boff:boff + (nwin - mi0) * 128], ALU.mult, eng=("pool" if (which == "win" and e == 0) else "dve"))
                    pend.append((c, u, js, pts, accs, i))
                    if len(pend) > 1:
                        finish(pend.pop(0))
            while pend:
                finish(pend.pop(0))

        for Q in range(4):
            do_cmp(Q)
            do_select(Q)
            do_slc(Q)
            do_band(Q, "win")
            do_band(Q, "swa")
        tap("selT", selT[0:32, :, :], [32, 2, SEQ], BF16)
        tap("selT2", selT[64:96, :, :], [32, 2, SEQ], BF16)
        tap("oa", oa[:, :, :, :], [128, NT, 8, 64], F32)
        tap("ob", ob[:, :, :, :], [128, NT, 8, 64], BF16)
        if stop == "attn":
            S.emit()
            return nc

        oaT = Buf(qaT.h, qaT.shape, BF16, "oaT", base_byte=qaT.base_byte, alias=ARENA)
        obT = Buf(qbT.h, qbT.shape, BF16, "obT", base_byte=qbT.base_byte, alias=ARENA)
        RM = Region(R_K.base, R_K.size + R_V.size + R_SEL.size + R_PT.size)
        RM.take("wgs01_reserved", [2, 8, 256], BF16)
        wa = RM.take("wa", [4, DM], BF16)
        wbm = RM.take("wbm", [4, DM], BF16)
        wo = RM.take("wo", [8, DM], BF16)
        wgs[2] = RM.take("wgs2", [8, 256], BF16)
        wgs[3] = RM.take("wgs3", [8, 256], BF16)
        oab = [RM.take("oab%d" % i, [512], BF16) for i in range(2)]
        sg = [RM.take("sg%d" % i, [512], F32) for i in range(2)]
        tA = RM.take("tA", [512], F32)
        tB = RM.take("tB", [512], F32)
        mixT = Buf(arena_h[:, R_OA.base // 2:(R_OA.base + 32768) // 2].rearrange("p (a b) -> p a b", a=8), [128, 8, SEQ], BF16,
                   "mixT", base_byte=R_OA.base, alias=ARENA)
        RX = Region(R_OB.base, R_OB.size)
        xr = [RX.take("xr%d" % i, [DM], F32) for i in range(2)]

        sqjunk = Buf(arena_h[:, sg[0].base_byte // 2:sg[0].base_byte // 2 + 1024], [128, DM], BF16, "sqjunk",
                     base_byte=sg[0].base_byte, alias=ARENA)
        S.dma(wa[:, :, :], wa_d.rearrange("(kc p) n -> p kc n", p=128), eng="pool")
        S.dma(wbm[:, :, :], wb_d.rearrange("(kc p) n -> p kc n", p=128), eng="pool")
        S.dma(wo[:, :, :], wo_d.rearrange("(kc p) n -> p kc n", p=128), eng="pool")
        obv = Buf(ob.h.rearrange("p t h d -> p t (h d)"), [128, NT, 512], BF16, "obv", base_byte=ob.base_byte, alias=ARENA)
        for t in range(NT):
            o_ = oab[t % 2]
            S.copy(o_[:, :], r3(oa[:, t, :, :], "p h d -> p (h d)"), eng="act")
            pb = PSB[t % 2]
            for c in range(4):
                S.transpose(pb[:, c * 128:(c + 1) * 128], o_[:, c * 128:(c + 1) * 128], identb)
            for c in range(4):
                S.transpose(pb[:, 512 + c * 128:512 + (c + 1) * 128], obv[:, t, c * 128:(c + 1) * 128], identb)
            S.copy(oaT[:, :, t * 128:(t + 1) * 128], r3(pb[:, 0:512], "p (c n) -> p c n", c=4), eng="dve")
            S.copy(obT[:, :, t * 128:(t + 1) * 128], r3(pb[:, 512:1024], "p (c n) -> p c n", c=4), eng="dve")
        n = 0
        for fo in range(8):
            if fo % 2 == 0:
                if fo // 2 + 1 < 4:
                    load_gate_piece(fo // 2 + 1)
            for tb in range(4):
                ts_ = slice(tb * 512, (tb + 1) * 512)
                res = []
                for ab, (wproj, oT, tdst) in enumerate(((wa, oaT, tA), (wbm, obT, tB))):
                    pg = PS[2 + n % 2]
                    pa = PS[4 + n % 2]
                    n += 1
                    for kc in range(8):
                        S.mm(pg[:, :], gpieces[(fo // 2, ab)][:, kc, (fo % 2) * 128:(fo % 2 + 1) * 128], hT[:, kc, ts_],
                             start=(kc == 0), stop=(kc == 7))
                    for ki in range(4):
                        S.mm(pa[:, :], wproj[:, ki, fo * 128:(fo + 1) * 128], oT[:, ki, ts_], start=(ki == 0), stop=(ki == 3))
                    s_ = sg[ab]
                    S.act(s_[:, :], pg[:, :], AF.Sigmoid)
                    S.tt(tdst[:, :], pa[:, :], s_[:, :], ALU.mult)
                S.tt(mixT[:, fo, ts_], tA[:, :], tB[:, :], ALU.add, eng="pool")
        x1 = Buf(arena_h[:, R_HT.base // 2:(R_HT.base + 65536) // 2].bitcast(F32).rearrange("p (t d) -> p t d", t=NT),
                 [128, NT, DM], F32, "x1", base_byte=R_HT.base, alias=ARENA)
        for t in range(NT):
            xs = xr[t % 2]
            S.dma(xs[:, :], x_d[t * 128:(t + 1) * 128, :])
            for dh in range(2):
                p = PS[6 + dh]
                for fo in range(8):
                    S.mm(p[:, :], mixT[:, fo, t * 128:(t + 1) * 128], wo[:, fo, dh * 512:(dh + 1) * 512],
                         start=(fo == 0), stop=(fo == 7))
                S.tt(x1[:, t, dh * 512:(dh + 1) * 512], p[:, :], xs[:, dh * 512:(dh + 1) * 512], ALU.add)
            S.act(sqjunk[:, :], x1[:, t, :], AF.Square, accum=ssq[:, t:t + 1])
        tap("x1", x1[:, :, :], [128, NT, DM], F32)
        if stop == "merge":
            S.emit()
            return nc

        h2T = Buf(arena_h[:, R_OA.base // 2:(R_OA.base + 32768) // 2].rearrange("p (a b) -> p a b", a=8), [128, 8, SEQ], BF16,
                  "h2T", base_byte=R_OA.base, alias=ARENA)
        RE = Region(R_K.base, R_K.size + R_V.size + R_SEL.size + R_PT.size)
        Wt = RE.take("Wt", [NT, 32], F32)
        wgu = [RE.take("wgu%d" % i, [2, 8, 256], BF16) for i in range(2)]
        wdn = [RE.take("wdn%d" % i, [2, DM], BF16) for i in range(3)]
        AT = [RE.take("AT%d" % i, [2, SEQ], BF16) for i in range(2)]
        sGs = [RE.take("sG%d" % i, [512], BF16) for i in range(2)]
        RR = Region(R_OB.base, R_OB.size)
        gbc2 = RR.take("gbc2", [DM], F32)
        h2f_a = RR.take("h2f", [DM], F32)
        h2b_a = RR.take("h2b", [DM], BF16)
        h2T32_a = RR.take("h2T32", [8, 128], F32)
        wr32 = RR.take("wr32", [8, 36], F32)
        RR2 = Region(AT[0].base_byte, 16384)
        h2f_b = RR2.take("h2f_b", [DM], F32)
        h2b_b = RR2.take("h2b_b", [DM], BF16)
        h2T32_b = RR2.take("h2T32_b", [8, 128], F32)
        h2b = h2b_a
        lgTs = [RR2.take("lgT%d" % i, [128], F32) for i in range(2)]
        RS = RE
        Lg = RS.take("Lg", [NT, 36], F32)
        bias36 = RS.take("bias36", [36], F32)
        ss2 = RS.take("ss2", [NT], F32)
        rstd2 = RS.take("rstd2", [NT], F32)
        rt = [RS.take("rt%d" % i, [NT], F32) for i in range(6)]
        gm4 = RS.take("gm4", [NT, 4], F32)
        ge4 = RS.take("ge4", [NT, 4], F32)
        m1 = RS.take("m1", [NT, 32], F32)
        m2 = RS.take("m2", [NT, 32], F32)
        em = RS.take("em", [NT, 32], F32)

        S.dma(gbc2[:, :], gffn_d.partition_broadcast(128))
        S.dma(wr32[:, :, 0:4], wgrp_d.rearrange("(kc p) n -> p kc n", p=128))
        S.dma(wr32[:, :, 4:36], wexp_d.rearrange("(kc p) n -> p kc n", p=128))
        S.dma(bias36[:, 0:4], bgrp_d.partition_broadcast(128))
        S.dma(bias36[:, 4:36], bexp_d.partition_broadcast(128))
        S.ts(rstd2[:, :], ssq[:, :], 1.0 / DM, 1e-6, ALU.mult, ALU.add)
        S.act(rstd2[:, :], rstd2[:, :], AF.Sqrt)
        S.recip(rstd2[:, :], rstd2[:, :])
        for t in range(NT):
            h2f, h2b, h2T32 = (h2f_a, h2b_a, h2T32_a) if t % 2 == 0 else (h2f_b, h2b_b, h2T32_b)
            S.stt(h2f[:, :], x1[:, t, :], rstd2[:, t:t + 1], gbc2[:, :], ALU.mult, ALU.mult)
            S.copy(h2b[:, :], h2f[:, :], eng="act")
            pb = PSB[t % 2]
            for c in range(8):
                S.transpose(pb[:, c * 128:(c + 1) * 128], h2b[:, c * 128:(c + 1) * 128], identb)
            S.copy(h2T[:, :, t * 128:(t + 1) * 128], r3(pb[:, 0:1024], "p (c n) -> p c n", c=8), eng="dve")
            for half in range(2):
                pf = PS[(2 if t % 2 == 0 else 6) + half]
                for c in range(4):
                    cc = half * 4 + c
                    S.transpose(pf[:, c * 128:(c + 1) * 128], h2f[:, cc * 128:(cc + 1) * 128], identf)
                S.copy(h2T32[:, half * 4:(half + 1) * 4, :], r3(pf[:, :], "p (c n) -> p c n", c=4), eng="act")
            pl = PS[4 + t % 2]
            for c in range(8):
                S.mm(pl[0:36, 0:128], wr32[:, c, :], h2T32[:, c, :], start=(c == 0), stop=(c == 7))
            lgT = lgTs[t % 2]
            S.copy(lgT[0:36, :], pl[0:36, 0:128], eng="act")
            S.transpose(pl[:, 256:292], lgT[0:36, :], CFs("identf", slice(0, 36), slice(0, 36)))
            S.tt(Lg[:, t, :], pl[:, 256:292], bias36[:, :], ALU.add)
        gl = Lg[:, :, 0:4]
        el = Lg[:, :, 4:36]
        gmax, gsum, top1, top2, w1_, a1 = rt
        S.reduce(gmax[:, :], gl, ALU.max)
        gmb = gmax[:, :].with_ap(lambda a: a.unsqueeze(2).broadcast_to([128, NT, 4]))
        S.tt(gm4[:, :, :], gl, gmb, ALU.is_equal)
        S.tt(ge4[:, :, :], gl, gmb, ALU.subtract)
        S.act(ge4[:, :, :], ge4[:, :, :], AF.Exp)
        S.reduce(gsum[:, :], ge4[:, :, :], ALU.add)
        S.recip(gsum[:, :], gsum[:, :])
        S.ts(gm4[:, :, :], gm4[:, :, :], -1.0, 1e30, ALU.add, ALU.mult)
        pen = gm4[:, :, :].with_ap(lambda a: a.unsqueeze(3).broadcast_to([128, NT, 4, 8]))
        S.tt(r3(em[:, :, :], "p t (g e) -> p t g e", g=4), r3(el, "p t (g e) -> p t g e", g=4), pen, ALU.add)
        S.reduce(top1[:, :], em[:, :, :], ALU.max)
        S.tt(m1[:, :, :], em[:, :, :], top1[:, :].with_ap(lambda a: a.unsqueeze(2).broadcast_to([128, NT, 32])), ALU.is_equal)
        S.stt(em[:, :, :], m1[:, :, :], -1e30, em[:, :, :], ALU.mult, ALU.add)
        S.reduce(top2[:, :], em[:, :, :], ALU.max)
        S.tt(m2[:, :, :], em[:, :, :], top2[:, :].with_ap(lambda a: a.unsqueeze(2).broadcast_to([128, NT, 32])), ALU.is_equal)
        S.tt(w1_[:, :], top2[:, :], top1[:, :], ALU.subtract)
        S.act(w1_[:, :], w1_[:, :], AF.Exp)
        S.ts(w1_[:, :], w1_[:, :], 1.0, None, ALU.add)
        S.recip(w1_[:, :], w1_[:, :])
        S.tt(a1[:, :], gsum[:, :], w1_[:, :], ALU.mult)
        S.tt(top2[:, :], gsum[:, :], a1[:, :], ALU.subtract)
        S.tt(m1[:, :, :], m1[:, :, :], a1[:, :].with_ap(lambda a: a.unsqueeze(2).broadcast_to([128, NT, 32])), ALU.mult)
        S.tt(m2[:, :, :], m2[:, :, :], top2[:, :].with_ap(lambda a: a.unsqueeze(2).broadcast_to([128, NT, 32])), ALU.mult)
        S.tt(Wt[:, :, :], m1[:, :, :], m2[:, :, :], ALU.add)
        tap("Wt", Wt[:, :, :], [128, NT, 32], F32)
        tap("Lg", Lg[:, :, :], [128, NT, 36], F32)
        if stop == "route":
            S.emit()
            return nc

        wg_v = wg_d.rearrange("e (kc p) f -> e p kc f", p=128)
        wu_v = wu_d.rearrange("e (kc p) f -> e p kc f", p=128)
        wd_v = wd_d.rearrange("e (fc p) n -> e p fc n", p=128)

        def load_expert(e):
            S.dma(wgu[e % 2][:, 0, :, :], wg_v[e], eng="pool")
            S.dma(wgu[e % 2][:, 1, :, :], wu_v[e], eng="pool")
            S.dma(wdn[e % 3][:, :, :], wd_v[e], eng="pool")

        NE = 32
        load_expert(0)
        n = 0
        ny = 0

        def down_units(e):
            A_ = AT[e % 2]
            wd_ = wdn[e % 3]
            units = []
            for t in range(NT):
                for dh in range(2):
                    units.append((e, A_, wd_, t, dh))
            return units

        def emit_down(unit):
            nonlocal ny
            e, A_, wd_, t, dh = unit
            py = PS[4 + ny % 4]
            ny += 1
            for fc in range(2):
                S.mm(py[:, :], A_[:, fc, t * 128:(t + 1) * 128], wd_[:, fc, dh * 512:(dh + 1) * 512],
                     start=(fc == 0), stop=(fc == 1))
            xv = x1[:, t, dh * 512:(dh + 1) * 512]
            S.stt(xv, py[:, :], Wt[:, t, e:e + 1], xv, ALU.mult, ALU.add)

        pending = []
        for e in range(NE):
            if e + 1 < NE:
                load_expert(e + 1)
            w = wgu[e % 2]
            A = AT[e % 2]
            for tb in range(4):
                ts_ = slice(tb * 512, (tb + 1) * 512)
                for fc in range(2):
                    pg = PS[0 + n % 2]
                    pu = PS[2 + n % 2]
                    n += 1
                    for kc in range(8):
                        S.mm(pg[:, :], w[:, 0, kc, fc * 128:(fc + 1) * 128], h2T[:, kc, ts_], start=(kc == 0), stop=(kc == 7))
                        if kc in (3, 7) and pending:
                            emit_down(pending.pop(0))
                    s_ = sGs[n % 2]
                    S.act(s_[:, :], pg[:, :], AF.Silu)
                    for kc in range(8):
                        S.mm(pu[:, :], w[:, 1, kc, fc * 128:(fc + 1) * 128], h2T[:, kc, ts_], start=(kc == 0), stop=(kc == 7))
                        if kc in (3, 7) and pending:
                            emit_down(pending.pop(0))
                    S.tt(A[:, fc, ts_], pu[:, :], s_[:, :], ALU.mult)
            while pending:
                emit_down(pending.pop(0))
            pending = down_units(e)
        RF = Region(R_OB.base, R_OB.size)
        gbc3 = RF.take("gbc3", [DM], F32)
        ot = [RF.take("ot%d" % i, [DM], F32) for i in range(2)]
        jk = RF.take("jk", [DM], BF16)
        S.dma(gbc3[:, :], gfin_d.partition_broadcast(128))
        while pending:
            unit = pending.pop(0)
            emit_down(unit)
            t, dh = unit[3], unit[4]
            if dh == 1:
                S.act(jk[:, :], x1[:, t, :], AF.Square, accum=ss2[:, t:t + 1])
                if t % 4 == 3:
                    t4 = t - 3
                    S.ts(rstd2[:, t4:t4 + 4], ss2[:, t4:t4 + 4], 1.0 / DM, 1e-6, ALU.mult, ALU.add)
                    S.act(rstd2[:, t4:t4 + 4], rstd2[:, t4:t4 + 4], AF.Sqrt)
                    S.recip(rstd2[:, t4:t4 + 4], rstd2[:, t4:t4 + 4])
                    for t_ in range(t4, t4 + 4):
                        o_ = ot[t_ % 2]
                        S.stt(o_[:, :], x1[:, t_, :], rstd2[:, t_:t_ + 1], gbc3[:, :], ALU.mult, ALU.mult)
                        S.dma(out_d[t_ * 128:(t_ + 1) * 128, :], o_[:, :], is_out=True)
        S.emit()
    return nc


_W_NAMES = ["norm_mix_g", "w_in", "cmp_pos_k", "cmp_w1_k", "cmp_w2_k", "cmp_pos_v", "cmp_w1_v", "cmp_w2_v", "sinks",
            "w_a", "w_b", "w_o", "norm_ffn_g", "w_group", "b_group", "w_expert", "b_expert",
            "w_gate_e", "w_up_e", "w_down_e"]


def make_in_maps(inputs, n_cores=8):
    cb, cf = make_consts()
    shared = {"constb": cb, "constf": cf}
    for k in _W_NAMES:
        a = np.ascontiguousarray(np.asarray(inputs[k], dtype=np.float32))
        shared[k] = a.reshape(a.shape[1:]) if a.shape[0] == 1 and a.ndim >= 2 else a
    for k in ("norm_mix_g", "sinks", "norm_ffn_g", "b_group", "b_expert"):
        shared[k] = shared[k].reshape(1, -1)
    shared["norm_final_g"] = np.ascontiguousarray(np.asarray(inputs["norm_final_g"], np.float32)).reshape(1, -1)
    x = np.asarray(inputs["x"], dtype=np.float32)
    return [dict(shared, x=np.ascontiguousarray(x[c])) for c in range(n_cores)]


_NC_CACHE = {}


def kernel(**inputs):
    if "nc" not in _NC_CACHE:
        _NC_CACHE["nc"] = build_program()
    nc = _NC_CACHE["nc"]
    in_maps = make_in_maps(inputs, 8)
    res = run_bass_kernel_spmd(nc, in_maps, core_ids=list(range(8)))
    out = np.stack([np.asarray(r["out"], dtype=np.float32) for r in res.results], axis=0)
    return out
```

```python
import contextlib
import ml_dtypes
from concourse.bass_utils import run_bass_kernel_spmd
import numpy as np
import concourse.bass as bass
import concourse.mybir as mybir

F32 = mybir.dt.float32
BF16 = mybir.dt.bfloat16
I32 = mybir.dt.int32
ALU = mybir.AluOpType
AF = mybir.ActivationFunctionType
AX = mybir.AxisListType

_DT_SIZE = {F32: 4, BF16: 2, I32: 4}


class View:
    __slots__ = ("buf", "ap", "box", "boxes")

    def __init__(self, buf, ap, box, boxes=None):
        self.buf = buf
        self.ap = ap
        self.box = box
        self.boxes = boxes if boxes is not None else [box]

    def with_ap(self, fn):
        return View(self.buf, fn(self.ap), self.box, self.boxes)


class Buf:
    def __init__(self, handle, shape, dtype, name, base_byte=0, alias=None):
        self.h = handle
        self.shape = tuple(shape)
        self.dtype = dtype
        self.name = name
        self.esz = _DT_SIZE[dtype]
        self.base_byte = base_byte
        self.alias = alias if alias is not None else self
        self.psum = False
        st = []
        s = 1
        for d in reversed(self.shape[1:]):
            st.append(s)
            s *= d
        self.fstrides = list(reversed(st))
        self.wr = []
        self.rd = {}
        self.rd_floor = {}
        self.wr_floor = 0

    def __getitem__(self, key):
        if not isinstance(key, tuple):
            key = (key,)
        key = list(key) + [slice(None)] * (len(self.shape) - len(key))
        k0 = key[0]
        if isinstance(k0, slice):
            p0, p1, _ = k0.indices(self.shape[0])
        else:
            p0, p1 = k0, k0 + 1
            key[0] = slice(k0, k0 + 1)
        lo = 0
        hi = 0
        for d, k in enumerate(key[1:]):
            n = self.shape[d + 1]
            if isinstance(k, slice):
                a, b, step = k.indices(n)
                cnt = len(range(a, b, step))
                last = a + (cnt - 1) * step
            else:
                a = k
                last = k
            lo += a * self.fstrides[d]
            hi += last * self.fstrides[d]
        box = (p0, p1, self.base_byte + lo * self.esz, self.base_byte + (hi + 1) * self.esz)
        if self.alias.psum:
            box = (0, 128, 0, 2048)
        return View(self.alias, self.h[tuple(key)], box)


def _overlap(a, b):
    return a[0] < b[1] and b[0] < a[1] and a[2] < b[3] and b[2] < a[3]


def _contains(outer, inner):
    return outer[0] <= inner[0] and outer[1] >= inner[1] and outer[2] <= inner[2] and outer[3] >= inner[3]


class Sched:
    ENGS = ("sync", "act", "dve", "pool", "pe")
    NSLOT = 6

    def __init__(self, nc):
        self.nc = nc
        self.ops = []
        self.eng_ops = {e: [] for e in self.ENGS}
        self.dma_count = {e: 0 for e in self.ENGS}
        self.dma_ops = {e: [] for e in self.ENGS}
        self.out_dmas = []
        self.debug = False

    MAXREC = 12

    def _is_inorder(self, oid):
        o = self.ops[oid]
        return not o["dma"]

    def op(self, eng, fn, reads=(), writes=(), dma=False, is_out=False):
        oid = len(self.ops)
        deps = set()
        reads = [(v.buf, bx) for v in reads for bx in v.boxes]
        writes = [(v.buf, bx) for v in writes for bx in v.boxes]
        for (vb, vbox) in reads:
            for (box, o) in vb.wr:
                if _overlap(box, vbox):
                    deps.add(o)
        for (vb, vbox) in writes:
            for (box, o) in vb.wr:
                if _overlap(box, vbox):
                    deps.add(o)
            for key, lst in vb.rd.items():
                if key == eng and not dma and eng == "pe":
                    continue
                for (box, o) in lst:
                    if _overlap(box, vbox):
                        deps.add(o)
        rec = dict(eng=eng, fn=fn, deps=deps, dma=dma, id=oid)
        if self.debug:
            import sys as _sys
            f = _sys._getframe(1)
            w = []
            while f is not None and len(w) < 4:
                if f.f_code.co_name not in ("op", "dma", "mm", "act", "tt", "ts", "stt", "copy", "memset", "recip", "reduce", "max8", "transpose"):
                    w.append("%s:%d" % (f.f_code.co_name, f.f_lineno))
                f = f.f_back
            rec["where"] = " < ".join(w)
        self.ops.append(rec)
        for (b, vbox) in writes:
            b.wr = [r for r in b.wr if not _contains(vbox, r[0])]
            for key in list(b.rd.keys()):
                b.rd[key] = [r for r in b.rd[key] if not _contains(vbox, r[0])]
            b.wr.append((vbox, oid))
            if len(b.wr) > 4 * self.MAXREC and len(b.wr) > 2 * b.wr_floor:
                self._merge_writes(b)
                b.wr_floor = len(b.wr)
        for (b, vbox) in reads:
            key = ("dma", oid) if dma else eng
            lst = b.rd.setdefault(key, [])
            lst[:] = [r for r in lst if r[0] != vbox]
            lst.append((vbox, oid))
            if len(lst) > self.MAXREC and len(lst) > 2 * b.rd_floor.get(key, 0):
                lst[:] = self._cluster(lst)
                b.rd_floor[key] = len(lst)
        if dma:
            i = self.dma_count[eng]
            self.dma_count[eng] += 1
            rec["dma_i"] = i
            if i >= self.NSLOT:
                deps.add(self.dma_ops[eng][i - self.NSLOT])
            self.dma_ops[eng].append(oid)
            if is_out:
                self.out_dmas.append(oid)
        rec["seq"] = len(self.eng_ops[eng])
        self.eng_ops[eng].append(oid)
        return oid

    @staticmethod
    def _cluster(lst, gap=512):
        lst = sorted(lst, key=lambda r: (r[0][0], r[0][1], r[0][2]))
        out = []
        for (box, o) in lst:
            if out:
                (pb, po) = out[-1]
                if pb[0] == box[0] and pb[1] == box[1] and box[2] <= pb[3] + gap:
                    out[-1] = ((pb[0], pb[1], pb[2], max(pb[3], box[3])), max(po, o))
                    continue
            out.append((box, o))
        return out

    def _merge_writes(self, b):
        groups = {}
        keep = []
        for (box, o) in b.wr:
            op_ = self.ops[o]
            if op_["dma"]:
                keep.append((box, o))
            else:
                groups.setdefault(op_["eng"], []).append((box, o))
        for e, lst in groups.items():
            if len(lst) <= 4:
                keep.extend(lst)
                continue
            keep.extend(self._cluster(lst))
        b.wr = keep

    def emit(self):
        nc = self.nc
        ops = self.ops
        fin = dict(eng="sync", fn=None, deps=set(self.out_dmas), dma=False, id=len(ops), seq=len(self.eng_ops["sync"]))
        self.eng_ops["sync"].append(fin["id"])
        ops.append(fin)
        raw_same = {}
        needed = [False] * len(ops)
        for o in ops:
            for d in o["deps"]:
                dop = ops[d]
                if dop["dma"]:
                    continue
                if dop["eng"] == o["eng"] and not o["dma"]:
                    if o["eng"] == "pe" or o["eng"] == "sync":
                        continue
                needed[d] = True
        cv = {}
        for e in self.ENGS:
            c = 0
            for oid in self.eng_ops[e]:
                o = ops[oid]
                if o["dma"] or o["fn"] is None:
                    continue
                if needed[oid]:
                    c += 1
                    cv[oid] = c
        import contextlib
        with contextlib.ExitStack() as st:
            sem_eng = {e: st.enter_context(nc.semaphore("s_" + e)) for e in ("act", "dve", "pool", "pe")}
            sem_dma = {e: [st.enter_context(nc.semaphore("d_%s%d" % (e, i))) for i in range(self.NSLOT)]
                       for e in ("sync", "act", "pool") if self.dma_count[e] > 0}
            block = st.enter_context(nc.Block())
            sched = self

            def run_engine(ename, e):
                seen = {x: 0 for x in ("act", "dve", "pool", "pe")}
                seen_dma = {}
                for oid in sched.eng_ops[ename]:
                    o = ops[oid]
                    for d in sorted(o["deps"]):
                        dop = ops[d]
                        if dop["dma"]:
                            q = dop["eng"]
                            i = dop["dma_i"]
                            slot = i % sched.NSLOT
                            val = 16 * (i // sched.NSLOT + 1)
                            if seen_dma.get((q, slot), 0) >= val:
                                continue
                            e.wait_ge(sem_dma[q][slot], val)
                            seen_dma[(q, slot)] = val
                        else:
                            de = dop["eng"]
                            if de == ename and not o["dma"] and ename in ("pe", "sync"):
                                continue
                            if d not in cv:
                                continue
                            val = cv[d]
                            if seen[de] >= val:
                                continue
                            e.wait_ge(sem_eng[de], val)
                            seen[de] = val
                    if o["fn"] is None:
                        continue
                    ins = o["fn"](e)
                    if sched.debug:
                        ins.annotate(o["where"])
                    if o["dma"]:
                        i = o["dma_i"]
                        ins.then_inc(sem_dma[ename][i % sched.NSLOT], 16)
                    elif needed[oid]:
                        ins.then_inc(sem_eng[ename], 1)

            @block.sync
            def _(e):
                run_engine("sync", e)

            @block.scalar
            def _(e):
                run_engine("act", e)

            @block.vector
            def _(e):
                run_engine("dve", e)

            @block.gpsimd
            def _(e):
                run_engine("pool", e)

            @block.tensor
            def _(e):
                run_engine("pe", e)

    def dma(self, out, in_, eng="sync", is_out=False, **kw):
        reads = [in_] if isinstance(in_, View) else []
        writes = [out] if isinstance(out, View) else []
        oa = out.ap if isinstance(out, View) else out
        ia = in_.ap if isinstance(in_, View) else in_
        return self.op(eng, lambda e: e.dma_start(out=oa, in_=ia, **kw), reads, writes, dma=True, is_out=is_out)

    def mm(self, out, lhsT, rhs, start=True, stop=True, **kw):
        return self.op("pe", lambda e: e.matmul(out.ap, lhsT.ap, rhs.ap, start=start, stop=stop, **kw),
                       [lhsT, rhs], [out])

    def transpose(self, out, in_, ident):
        return self.op("pe", lambda e: e.transpose(out.ap, in_.ap, ident.ap), [in_, ident], [out])

    def act(self, out, in_, func, bias=None, scale=None, accum=None, eng="act"):
        reads = [in_]
        kw = {}
        if bias is not None:
            if isinstance(bias, View):
                reads.append(bias)
                kw["bias"] = bias.ap
            else:
                kw["bias"] = bias
        if scale is not None:
            if isinstance(scale, View):
                reads.append(scale)
                kw["scale"] = scale.ap
            else:
                kw["scale"] = scale
        writes = [out]
        if accum is not None:
            writes.append(accum)
            kw["accum_out"] = accum.ap
        return self.op(eng, lambda e: e.activation(out.ap, in_.ap, func, **kw), reads, writes)

    def tt(self, out, in0, in1, op, eng="dve"):
        return self.op(eng, lambda e: e.tensor_tensor(out.ap, in0.ap, in1.ap, op), [in0, in1], [out])

    def ts(self, out, in0, s1, s2, op0, op1=None, eng="dve", accum=None):
        reads = [in0]
        a1 = s1
        a2 = s2
        if isinstance(s1, View):
            reads.append(s1)
            a1 = s1.ap
        if isinstance(s2, View):
            reads.append(s2)
            a2 = s2.ap
        writes = [out]
        kw = {}
        if op1 is not None:
            kw["op1"] = op1
        if accum is not None:
            writes.append(accum)
            kw["accum_out"] = accum.ap
        return self.op(eng, lambda e: e.tensor_scalar(out.ap, in0.ap, a1, a2, op0, **kw), reads, writes)

    def stt(self, out, in0, scalar, in1, op0, op1, eng="dve"):
        reads = [in0, in1]
        sc = scalar
        if isinstance(scalar, View):
            reads.append(scalar)
            sc = scalar.ap
        return self.op(eng, lambda e: e.scalar_tensor_tensor(out.ap, in0.ap, sc, in1.ap, op0, op1), reads, [out])

    def copy(self, out, in_, eng="dve"):
        if eng == "act":
            return self.op(eng, lambda e: e.copy(out.ap, in_.ap), [in_], [out])
        return self.op(eng, lambda e: e.tensor_copy(out.ap, in_.ap), [in_], [out])

    def memset(self, out, val, eng="pool"):
        return self.op(eng, lambda e: e.memset(out.ap, val), [], [out])

    def recip(self, out, in_):
        return self.op("dve", lambda e: e.reciprocal(out.ap, in_.ap), [in_], [out])

    def reduce(self, out, in_, op, axis=AX.X, eng="dve"):
        return self.op(eng, lambda e: e.tensor_reduce(out.ap, in_.ap, axis, op), [in_], [out])

    def max8(self, out, in_):
        return self.op("dve", lambda e: e.max(out.ap, in_.ap), [in_], [out])


SEQ = 2048
DM = 1024
NT = 16
NEG = -30000.0
BF = ml_dtypes.bfloat16


def _slopes():
    n = 16
    s = 2.0 ** (-8.0 * np.arange(1, n + 1) / n)
    return s[:8].astype(np.float64), s[8:].astype(np.float64)


CB = {}
CF = {}


def _layout():
    o = 0
    for name, n in (("identb", 128), ("esel", SEQ), ("cmpmask", SEQ), ("ovp", 33), ("tri", 128),
                    ("band_win", 8 * 3 * 128), ("band_swa", 8 * 2 * 128)):
        CB[name] = (o, n)
        o += n
    CB["_n"] = o + (o % 2)
    o = 0
    for name, n in (("identf", 128), ("forced", NT * 32), ("bias_cmp", 32), ("bias_slc", 8 * 19),
                    ("bias_band", 16), ("sq_swa", 8)):
        CF[name] = (o, n)
        o += n
    CF["_n"] = o


_layout()


def make_consts():
    s_swa, s_nsa = _slopes()
    cb = np.zeros((128, CB["_n"]), np.float32)
    cf = np.zeros((128, CF["_n"]), np.float32)

    def putb(name, arr):
        c0, n = CB[name]
        arr = np.asarray(arr, np.float32).reshape(arr.shape[0], -1)
        assert arr.shape[1] == n, (name, arr.shape, n)
        cb[:arr.shape[0], c0:c0 + n] = arr

    def putf(name, arr):
        c0, n = CF[name]
        arr = np.asarray(arr, np.float32).reshape(arr.shape[0], -1)
        assert arr.shape[1] == n, (name, arr.shape, n)
        cf[:arr.shape[0], c0:c0 + n] = arr

    putb("identb", np.eye(128))
    putf("identf", np.eye(128))
    k = np.arange(SEQ)
    putb("esel", (k[None, :] // 64 == np.arange(32)[:, None]).astype(np.float32))
    c = np.arange(127)
    endc = 16 * c + 31
    putb("cmpmask", np.where(endc[:, None] <= k[None, :], 0.0, NEG))
    c0 = c * 16
    s0 = np.arange(32) * 64
    ov = np.clip(np.minimum(c0[:, None] + 32, s0[None, :] + 64) - np.maximum(c0[:, None], s0[None, :]), 0, None) / 32.0
    putb("ovp", np.concatenate([ov, np.ones((127, 1))], axis=1))
    kk = np.arange(128)
    putb("tri", (kk[:, None] <= kk[None, :]).astype(np.float32))
    def band_f(slopes, nwin, window):
        f = np.zeros((128, 8, nwin, 128))
        for h in range(8):
            for mi in range(nwin):
                m = nwin - 1 - mi
                dist = 128 * m + kk[None, :] - kk[:, None]
                e_ = -slopes[h] * dist - slopes[h] * (kk[:, None] - 127) / 2.0
                f[:, h, mi, :] = np.where((dist >= 0) & (dist < window), np.exp(np.minimum(e_, 80.0)), 0.0)
        return f
    putb("band_win", band_f(s_nsa, 3, 256))
    putb("band_swa", band_f(s_swa, 2, 128))
    tq = (np.arange(NT)[None, :] * 128 + kk[:, None])
    cur = tq // 64
    j = np.arange(32)[None, None, :]
    forced = ((j == 0) | (j == cur[:, :, None]) | (j == cur[:, :, None] - 1))
    putf("forced", np.where(forced, 1e9, 0.0))
    bc = np.zeros((128, 8, 4))
    for h in range(8):
        for Q in range(4):
            bc[:127, h, Q] = s_nsa[h] * (endc - (512 * Q + 256))
    putf("bias_cmp", bc)
    bsl = np.zeros((128, 8, 19))
    for h in range(8):
        for dk in range(-15, 4):
            bsl[:, h, dk + 15] = s_nsa[h] * (128 * dk + kk - 256)
    putf("bias_slc", bsl)
    bb = np.zeros((128, 16))
    for h in range(8):
        bb[:, h] = s_nsa[h] * (kk - 127) / 2.0
        bb[:, 8 + h] = s_swa[h] * (kk - 127) / 2.0
    putf("bias_band", bb)
    putf("sq_swa", np.zeros((128, 8)))
    return cb.astype(BF), cf.astype(np.float32)


C_QA, C_KCMP, C_VCMP, C_KSLC, C_VSLC, C_KWIN, C_VWIN, C_GNSA, C_QB, C_KB, C_VB, C_GM = (
    0, 512, 640, 768, 896, 1024, 1152, 1280, 1304, 1816, 1944, 2072)


def build_program(dbg=None, stop=None):
    dbg = dbg or []
    nc = bass.Bass("TRN2", target_bir_lowering=False)

    def din(name, shape, dt=F32):
        return nc.dram_tensor(name, list(shape), dt, kind="ExternalInput").ap()

    x_d = din("x", [SEQ, DM])
    constb_d = din("constb", [128, CB["_n"]], BF16)
    constf_d = din("constf", [128, CF["_n"]])
    gmix_d = din("norm_mix_g", [1, DM])
    win_d = din("w_in", [DM, 4120])
    posk_d = din("cmp_pos_k", [32, 64])
    w1k_d = din("cmp_w1_k", [2048, 256])
    w2k_d = din("cmp_w2_k", [256, 64])
    posv_d = din("cmp_pos_v", [32, 64])
    w1v_d = din("cmp_w1_v", [2048, 256])
    w2v_d = din("cmp_w2_v", [256, 64])
    sinks_d = din("sinks", [1, 8])
    wa_d = din("w_a", [512, DM])
    wb_d = din("w_b", [512, DM])
    wo_d = din("w_o", [DM, DM])
    gffn_d = din("norm_ffn_g", [1, DM])
    wgrp_d = din("w_group", [DM, 4])
    bgrp_d = din("b_group", [1, 4])
    wexp_d = din("w_expert", [DM, 32])
    bexp_d = din("b_expert", [1, 32])
    wg_d = din("w_gate_e", [32, DM, 256])
    wu_d = din("w_up_e", [32, DM, 256])
    wd_d = din("w_down_e", [32, 256, DM])
    gfin_d = din("norm_final_g", [1, DM])
    out_d = nc.dram_tensor("out", [SEQ, DM], F32, kind="ExternalOutput").ap()

    ARENA_BYTES = 212000
    with contextlib.ExitStack() as st:
        arena_h = st.enter_context(nc.sbuf_tensor("arena", [128, ARENA_BYTES // 2], BF16))
        ps_h = [st.enter_context(nc.psum_tensor("ps%d" % i, [128, 512], F32)) for i in range(8)]
        S = Sched(nc)
        S.debug = bool(globals().get('DEBUG_SCHED', False))
        ARENA = Buf(arena_h, [128, ARENA_BYTES // 2], BF16, "arena")

        def carve(name, fshape, dtype, at):
            fshape = list(fshape)
            n = int(np.prod(fshape))
            nb = n * _DT_SIZE[dtype]
            assert at % 4 == 0 and at + nb <= ARENA_BYTES, (name, at, nb)
            ap = arena_h[:, at // 2:(at + nb) // 2]
            if dtype == F32:
                ap = ap.bitcast(F32)
            if len(fshape) == 2:
                ap = ap.rearrange("p (a b) -> p a b", a=fshape[0])
            elif len(fshape) == 3:
                ap = ap.rearrange("p (a b c) -> p a b c", a=fshape[0], b=fshape[1])
            elif len(fshape) == 4:
                ap = ap.rearrange("p (a b c d) -> p a b c d", a=fshape[0], b=fshape[1], c=fshape[2])
            b = Buf(ap, [128] + fshape, dtype, name, base_byte=at, alias=ARENA)
            b.nbytes = nb
            return b

        class Region:
            def __init__(self, base, size):
                self.base, self.size, self.off = base, size, 0

            def take(self, name, fshape, dtype):
                self.off = (self.off + 3) // 4 * 4
                b = carve(name, fshape, dtype, self.base + self.off)
                self.off += b.nbytes
                assert self.off <= self.size, (name, self.off, self.size)
                return b

        PS = [Buf(p, [128, 512], F32, "ps%d" % i) for i, p in enumerate(ps_h)]
        for p_ in PS:
            p_.psum = True
        PSB = [Buf(p.bitcast(BF16), [128, 1024], BF16, "psb%d" % i, alias=PS[i]) for i, p in enumerate(ps_h)]

        def r3(view, pat, **kw):
            return view.with_ap(lambda a: a.rearrange(pat, **kw))

        _rb = [0]

        def _reg(size):
            r = Region(_rb[0], size)
            _rb[0] += size
            return r

        R_CONST = _reg(30720)
        R_HT = _reg(32768)
        R_Q = _reg(32768)
        R_K = _reg(32768)
        R_V = _reg(12544)
        R_SEL = _reg(8192)
        R_PT = _reg(12288)
        R_OA = _reg(32768)
        R_OB = _reg(16384)
        assert _rb[0] <= ARENA_BYTES, _rb[0]

        constb = R_CONST.take("constb", [CB["_n"]], BF16)
        constf = R_CONST.take("constf", [CF["_n"]], F32)
        gnsa = R_CONST.take("gnsa", [NT, 24], F32)
        ssq = R_CONST.take("ssq", [NT], F32)
        rstd = R_CONST.take("rstd", [NT], F32)
        kcT = R_CONST.take("kcT", [2, 128], BF16)
        vcp = R_CONST.take("vcp", [2, 65], BF16)
        sinkt = R_CONST.take("sinkt", [8], F32)
        smallf = R_CONST.take("smallf", [64], F32)

        def cbv(name, p0=0, p1=128):
            c0, n = CB[name]
            return constb, c0, n

        def CBs(name, rows=slice(0, 128), cols=None):
            c0, n = CB[name]
            if cols is None:
                cols = slice(0, n)
            return constb[rows, c0 + cols.start:c0 + cols.stop]

        def CFs(name, rows=slice(0, 128), cols=None):
            c0, n = CF[name]
            if cols is None:
                cols = slice(0, n)
            return constf[rows, c0 + cols.start:c0 + cols.stop]

        identb = CBs("identb")
        identf = CFs("identf")

        hT = R_HT.take("hT", [8, SEQ], BF16)
        qaT = R_Q.take("qaT", [4, SEQ], BF16)
        qbT = R_Q.take("qbT", [4, SEQ], BF16)
        kcmpT = R_K.take("kcmpT", [SEQ], BF16)
        vcmpT = R_K.take("vcmpT", [SEQ], BF16)
        kslc = R_K.take("kslc", [2, SEQ], BF16)
        kwin = R_K.take("kwin", [2, SEQ], BF16)
        kb = R_K.take("kb", [2, SEQ], BF16)
        vtm = R_V.take("vtm", [NT, 6, 65], BF16)
        selT = R_SEL.take("selT", [2, SEQ], BF16)
        NPT = 6
        PTb = [R_PT.take("pt%d" % i, [512], BF16) for i in range(NPT)]
        evs = R_PT.take("evs", [512], F32)
        oa = R_OA.take("oa", [NT, 8, 64], F32)
        ob = R_OB.take("ob", [NT, 8, 64], BF16)

        debug_outs = []

        def tap(name, buf_view, shape, dt=F32):
            if name not in dbg:
                return
            d = nc.dram_tensor("dbg_" + name, list(shape), dt, kind="ExternalOutput").ap()
            S.dma(d, buf_view, is_out=True)

        S.dma(constb[:, :], constb_d)
        S.dma(constf[:, :], constf_d)

        xall = Buf(arena_h[:, R_Q.base // 2:(R_Q.base + 65536) // 2].bitcast(F32).rearrange("p (t d) -> p t d", t=NT),
                   [128, NT, DM], F32, "xall", base_byte=R_Q.base, alias=ARENA)
        PW = Region(R_OA.base, R_OA.size)
        wst = [PW.take("wst%d" % i, [8, 128], BF16) for i in range(16)]
        P0 = Region(R_OB.base, R_OB.size)
        wtm = P0.take("wtm", [8, 408], BF16)
        gbc = P0.take("gbc", [DM], F32)
        hb = [P0.take("hb%d" % i, [DM], BF16) for i in range(2)]
        PJ = Region(R_SEL.base, R_SEL.size + R_PT.size)
        w1k_buf = PJ.take("w1k", [32, 256], BF16)
        junk = PJ.take("junk", [DM], BF16)

        win_v = win_d.rearrange("(kc p) n -> p kc n", p=128)
        S.dma(gbc[:, :], gmix_d.partition_broadcast(128))

        def xtile(t):
            a_, b_ = t // 4, t % 4
            lo = (4 * b_) * 4096 + a_ * 1024
            hi = (4 * b_ + 3) * 4096 + a_ * 1024 + 1024
            ap = xall.h.rearrange("p s (q c) -> p s q c", q=4)[:, 4 * b_:4 * b_ + 4, a_, :]
            boxes = [(0, 128, R_Q.base + (4 * b_ + j) * 4096 + a_ * 1024, R_Q.base + (4 * b_ + j) * 4096 + a_ * 1024 + 1024)
                     for j in range(4)]
            return View(ARENA, ap, (0, 128, R_Q.base + lo, R_Q.base + hi), boxes)

        for t in range(NT):
            S.dma(xtile(t), x_d[t * 128:(t + 1) * 128, :].rearrange("p (j c) -> p j c", j=4))
        fm = []
        for c in range(4):
            fm.append((lambda tb, c=c: qaT[:, c, tb * 512:(tb + 1) * 512], [(C_QA + c * 128, 128)], 0.125))
        fm.append((lambda tb: kcmpT[:, tb * 512:(tb + 1) * 512], [(C_KCMP, 128)], 1.0))
        fm.append((lambda tb: vcmpT[:, tb * 512:(tb + 1) * 512], [(C_VCMP, 128)], 1.0))
        for (dst, c0) in ((kslc, C_KSLC), (kwin, C_KWIN)):
            for g in range(2):
                fm.append((lambda tb, dst=dst, g=g: dst[:, g, tb * 512:(tb + 1) * 512],
                           [(c0 + g * 64, 64), (c0 + g * 64, 64)], 1.0))
        for c in range(4):
            fm.append((lambda tb, c=c: qbT[:, c, tb * 512:(tb + 1) * 512], [(C_QB + c * 128, 128)], 0.125))
        for g in range(2):
            fm.append((lambda tb, g=g: kb[:, g, tb * 512:(tb + 1) * 512],
                       [(C_KB + g * 64, 64), (C_KB + g * 64, 64)], 1.0))
        for ci, (dstf, pieces, scale) in enumerate(fm):
            o = 0
            for (c0, n) in pieces:
                S.dma(wst[ci][:, :, o:o + n], win_v[:, :, c0:c0 + n], eng="pool")
                o += n
        for i, c0 in enumerate((C_VSLC, C_VWIN, C_VB)):
            S.dma(wtm[:, :, i * 128:(i + 1) * 128], win_v[:, :, c0:c0 + 128], eng="pool")
        S.dma(wtm[:, :, 384:408], win_v[:, :, C_GNSA:C_GNSA + 24], eng="pool")
        w1kv_ = w1k_d.rearrange("(l d) h -> d l h", d=64)
        S.dma(w1k_buf[0:64, :, :], w1kv_, eng="pool")
        S.dma(w1k_buf[64:128, :, :], w1kv_, eng="pool")

        def p0_group(t4):
            j3 = r3(junk[:, :], "p (j c) -> p j c", j=4)
            for t in range(t4, t4 + 4):
                S.act(j3, xtile(t), AF.Square, accum=ssq[:, t:t + 1])
            S.ts(rstd[:, t4:t4 + 4], ssq[:, t4:t4 + 4], 1.0 / DM, 1e-6, ALU.mult, ALU.add)
            S.act(rstd[:, t4:t4 + 4], rstd[:, t4:t4 + 4], AF.Sqrt)
            S.recip(rstd[:, t4:t4 + 4], rstd[:, t4:t4 + 4])
            for t in range(t4, t4 + 4):
                h = hb[t % 2]
                S.stt(r3(h[:, :], "p (j c) -> p j c", j=4), xtile(t), rstd[:, t:t + 1],
                      r3(gbc[:, :], "p (j c) -> p j c", j=4), ALU.mult, ALU.mult)
                pb = PSB[t % 2]
                for c in range(8):
                    S.transpose(pb[:, c * 128:(c + 1) * 128], h[:, c * 128:(c + 1) * 128], identb)
                S.copy(hT[:, :, t * 128:(t + 1) * 128], r3(pb[:, 0:1024], "p (c n) -> p c n", c=8),
                       eng=("act" if t % 2 else "dve"))

        pj = {"nev": 0}

        def proj_block(tb):
            for ci, (dstf, pieces, scale) in enumerate(fm):
                w = wst[ci]
                p = PS[2 + (pj["nev"] % 4)]
                for kc in range(8):
                    S.mm(p[:, :], w[:, kc, :], hT[:, kc, tb * 512:(tb + 1) * 512], start=(kc == 0), stop=(kc == 7))
                if pj["nev"] % 2 == 0:
                    S.act(dstf(tb), p[:, :], AF.Copy, scale=scale)
                else:
                    S.ts(dstf(tb), p[:, :], scale, None, ALU.mult)
                pj["nev"] += 1
            for t in range(4 * tb, 4 * tb + 4):
                p = PS[6 + t % 2]
                for kc in range(8):
                    S.mm(p[:, 0:408], hT[:, kc, t * 128:(t + 1) * 128], wtm[:, kc, :], start=(kc == 0), stop=(kc == 7))
                S.copy(vtm[:, t, :, 0:64], r3(p[:, 0:384], "p (s d) -> p s d", s=6), eng="dve")
                S.act(gnsa[:, t, :], p[:, 384:408], AF.Sigmoid)

        S.memset(vtm[:, :, :, 64:65], 1.0)
        p0_group(0)
        p0_group(4)
        p0_group(8)
        p0_group(12)
        proj_block(0)
        proj_block(1)
        proj_block(2)
        proj_block(3)
        tap("hT", hT[:, :, :], [128, 8, SEQ], BF16)
        if stop == "p0":
            S.emit()
            return nc

        for nm, b, sh in (("qaT", qaT, [128, 4, SEQ]), ("qbT", qbT, [128, 4, SEQ]), ("kslc", kslc, [128, 2, SEQ]),
                          ("kcmpT", kcmpT, [128, SEQ]), ("vtm", vtm, [128, NT, 6, 65])):
            tap(nm, b[(slice(None),) * len(sh)], sh, BF16)
        tap("gnsa", gnsa[:, :, :], [128, NT, 24], F32)
        if stop == "proj":
            S.emit()
            return nc

        PC = Region(R_OA.base, R_OA.size + R_OB.size)
        w1d = {"k": w1k_buf, "v": PC.take("w1v", [32, 256], BF16)}
        w2kd = PC.take("w2kd", [2, 128], BF16)
        w2v = PC.take("w2v", [2, 64], BF16)
        posT = {"k": PC.take("posTk", [32], BF16), "v": PC.take("posTv", [32], BF16)}
        posb = {"k": PC.take("posbk", [2], F32), "v": PC.take("posbv", [2], F32)}
        hidT = {"k": PC.take("hidTk", [2, 2, 128], BF16), "v": PC.take("hidTv", [2, 2, 128], BF16)}
        for X, w1_d, pos_d in (("k", w1k_d, posk_d), ("v", w1v_d, posv_d)):
            if X == "v":
                w1v_ = w1_d.rearrange("(l d) h -> d l h", d=64)
                S.dma(w1d[X][0:64, :, :], w1v_, eng="pool")
                S.dma(w1d[X][64:128, :, :], w1v_, eng="pool")
            S.dma(posT[X][0:64, :], pos_d.rearrange("l d -> d l"), eng="pool", allow_slow_non_contiguous=True)
        w2k_v = w2k_d.rearrange("(hc p) d -> p hc d", p=128)
        S.dma(w2kd[:, :, 0:64], w2k_v, eng="pool")
        S.dma(w2kd[:, :, 64:128], w2k_v, eng="pool")
        S.dma(w2v[:, :, :], w2v_d.rearrange("(hc p) d -> p hc d", p=128), eng="pool")
        S.memset(vcp[:, :, 64:65], 1.0)
        for X, srcT in (("k", kcmpT), ("v", vcmpT)):
            pp = PS[0]
            for hc in range(2):
                for l in range(32):
                    S.mm(pp[:, hc:hc + 1], w1d[X][0:64, l, hc * 128:(hc + 1) * 128], posT[X][0:64, l:l + 1],
                         start=(l == 0), stop=(l == 31))
            S.copy(posb[X][:, :], pp[:, 0:2])
            for hc in range(2):
                pg_ = [PS[1 + 2 * hc], PS[2 + 2 * hc]]
                for l in range(32):
                    for g in range(2):
                        S.mm(pg_[g][:, 0:127], w1d[X][64 * g:64 * g + 64, l, hc * 128:(hc + 1) * 128],
                             srcT[64 * g:64 * g + 64, l:l + 16 * 126 + 1:16], start=(l == 0), stop=(l == 31))
                for g in range(2):
                    S.act(hidT[X][:, g, hc, 0:127], pg_[g][:, 0:127], AF.Silu, bias=posb[X][:, hc:hc + 1])
        for g in range(2):
            p = PS[5 + g]
            for hc in range(2):
                S.mm(p[:, g * 128:g * 128 + 127], w2kd[:, hc, :], hidT["k"][:, g, hc, 0:127], start=(hc == 0), stop=(hc == 1))
            S.copy(kcT[:, g, 0:127], p[:, g * 128:g * 128 + 127])
        for g in range(2):
            p = PS[6 + g]
            for hc in range(2):
                S.mm(p[0:127, g * 64:(g + 1) * 64], hidT["v"][:, g, hc, 0:127], w2v[:, hc, :], start=(hc == 0), stop=(hc == 1))
            S.copy(vcp[0:127, g, 0:64], p[0:127, g * 64:(g + 1) * 64], eng="act")
        RG0 = Region(R_K.base, 8192)
        wgs = [RG0.take("wgs%d" % i, [8, 256], BF16) for i in range(2)]
        wgs += [None, None]
        gpieces = {}

        def load_gate_piece(fp):
            for ab in range(2):
                w = wgs[(2 * fp + ab) % 4]
                c0 = C_GM + ab * 1024 + fp * 256
                S.dma(w[:, :, :], win_v[:, :, c0:c0 + 256], eng="pool")
                gpieces[(fp, ab)] = w

        load_gate_piece(0)
        tap("kcT", kcT[:, :, 0:127], [128, 2, 127], BF16)
        tap("vcp", vcp[0:127, :, :], [127, 2, 65], BF16)
        if stop == "cmpmlp":
            S.emit()
            return nc

        sk = smallf[:, 0:8]
        S.dma(sk, sinks_d.partition_broadcast(128))
        S.tt(sk, sk, CFs("sq_swa"), ALU.add)
        S.act(sinkt[:, :], sk, AF.Exp)

        imp_sb = R_PT.take("imp_sb", [4, 2, 32], F32)
        selb = R_PT.take("selb", [4, 2, 32], BF16)
        Msb = [R_PT.take("msb%d" % i, [512], BF16) for i in range(2)]
        top8 = R_PT.take("top8", [8], F32)
        ev_rz = evs[:, 0:4]
        ev_coef = evs[:, 4:8]
        ev_rzi = evs[:, 8:12]
        ev_tmp = evs[:, 64:64 + 256]
        ev_tmp2 = evs[:, 320:320 + 128]
        S.memset(selT[:, :, :], 0.0)

        st_banks = [0, 1, 2, 3]
        cnt = {"st": 0, "pt": 0}

        def next_st():
            b_ = PS[st_banks[cnt["st"] % 4]]
            cnt["st"] += 1
            return b_

        def next_pt():
            b_ = PTb[cnt["pt"] % NPT]
            cnt["pt"] += 1
            return b_

        def accv(p, u0, u1, d0, d1):
            v = p[:, u0 * 65:(u1 - 1) * 65 + d1]
            ap = p.h[:, 0:260].rearrange("p (u d) -> p u d", u=4)[:, u0:u1, d0:d1]
            return View(v.buf, ap, v.box)

        def acc1(p, u):
            return accv(p, u, u + 1, 0, 65).with_ap(lambda a: a[:, 0, :])

        def evac_nsa(p, Q, h, br, first):
            z = accv(p, 0, 4, 64, 65)
            S.ts(ev_rz.with_ap(lambda a: a.unsqueeze(2)), z, 1e-30, None, ALU.max)
            S.recip(ev_rz, ev_rz)
            gate = gnsa[:, 4 * Q:4 * Q + 4, h * 3 + br]
            S.tt(ev_coef, ev_rz, gate, ALU.mult)
            cb_ = ev_coef.with_ap(lambda a: a.unsqueeze(2).broadcast_to([128, 4, 64]))
            o = oa[:, 4 * Q:4 * Q + 4, h, :]
            if first:
                S.tt(o, accv(p, 0, 4, 0, 64), cb_, ALU.mult)
            else:
                t3 = r3(ev_tmp, "p (u d) -> p u d", u=4)
                S.tt(t3, accv(p, 0, 4, 0, 64), cb_, ALU.mult)
                S.tt(o, o, t3, ALU.add, eng="pool")

        HALF = ((0, 64), (64, 128))

        def do_cmp(Q):
            qs = slice(Q * 512, (Q + 1) * 512)
            pend = []

            def finish(step):
                c, pts = step
                g = c // 2
                hs = (2 * c, 2 * c + 1)
                for e in range(2):
                    accp = PS[4 + e]
                    impp = PS[6 + e]
                    pt = pts[e]
                    for u in range(4):
                        S.mm(acc1(accp, u), pt[0:127, u * 128:(u + 1) * 128], vcp[0:127, g, :],
                             start=(u == 0), stop=True, skip_group_check=True)
                    for u in range(4):
                        S.mm(impp[:, u * 33:(u + 1) * 33], pt[0:127, u * 128:(u + 1) * 128], CBs("ovp", slice(0, 127)),
                             start=(u == 0), stop=True, skip_group_check=True)
                for e in range(2):
                    h = hs[e]
                    accp = PS[4 + e]
                    impp = PS[6 + e]
                    evac_nsa(accp, Q, h, 0, True)
                    zi = View(impp, impp.h[:, 0:132].rearrange("p (u d) -> p u d", u=4)[:, :, 32:33], impp[:, 0:132].box)
                    S.ts(ev_rzi.with_ap(lambda a: a.unsqueeze(2)), zi, 1e-30, None, ALU.max)
                    S.recip(ev_rzi, ev_rzi)
                    rb = ev_rzi.with_ap(lambda a: a.unsqueeze(2).broadcast_to([128, 4, 32]))
                    iv = View(impp, impp.h[:, 0:132].rearrange("p (u d) -> p u d", u=4)[:, :, 0:32], impp[:, 0:132].box)
                    if h % 4 == 0:
                        S.tt(imp_sb[:, :, g, :], iv, rb, ALU.mult)
                    else:
                        t3 = r3(ev_tmp2, "p (u d) -> p u d", u=4)
                        S.tt(t3, iv, rb, ALU.mult)
                        S.tt(imp_sb[:, :, g, :], imp_sb[:, :, g, :], t3, ALU.add, eng="pool")

            for c in range(4):
                g = c // 2
                hs = (2 * c, 2 * c + 1)
                stp = [next_st(), next_st()]
                for e in range(2):
                    lo, hi = HALF[e]
                    S.mm(stp[e][0:127, :], kcT[lo:hi, g, 0:127], qaT[lo:hi, c, qs], start=True, stop=False)
                for e in range(2):
                    S.mm(stp[e][0:127, :], CBs("identb", slice(0, 127), slice(0, 127)), CBs("cmpmask", slice(0, 127), qs),
                         start=False, stop=True)
                pts = [next_pt(), next_pt()]
                for e in range(2):
                    h = hs[e]
                    S.act(pts[e][0:127, :], stp[e][0:127, :], AF.Exp,
                          bias=CFs("bias_cmp", slice(0, 127), slice(h * 4 + Q, h * 4 + Q + 1)))
                pend.append((c, pts))
                if len(pend) > 1:
                    finish(pend.pop(0))
            while pend:
                finish(pend.pop(0))

        def do_select(Q):
            c0, _ = CF["forced"]
            fv = View(constf.alias, constf.h[:, c0 + 4 * Q * 32:c0 + (4 * Q + 4) * 32].rearrange("p (u j) -> p u j", u=4)
                      .unsqueeze(2).broadcast_to([128, 4, 2, 32]), constf[:, c0 + 4 * Q * 32:c0 + (4 * Q + 4) * 32].box)
            S.tt(imp_sb[:, :, :, :], imp_sb[:, :, :, :], fv, ALU.max)
            for g in range(2):
                pb = PSB[6 + g]
                for u in range(4):
                    S.max8(top8[:, :], imp_sb[:, u, g, :])
                    S.ts(selb[:, u, g, :], imp_sb[:, u, g, :], top8[:, 7:8], None, ALU.is_ge)
                    S.transpose(pb[0:32, u * 128:(u + 1) * 128], selb[:, u, g, :], identb)
                S.copy(selT[0:32, g, Q * 512:(Q + 1) * 512], pb[0:32, 0:512], eng="act")

        msb_cnt = [0]

        def do_slc(Q):
            nk = 4 * Q + 4
            for g in range(2):
                cs = (2 * g, 2 * g + 1)
                accs = {cs[0]: [PS[4], PS[5]], cs[1]: [PS[6], PS[7]]}
                pend = []

                def pv(kt, c, pts):
                    r = kt - 4 * Q
                    for e in range(2):
                        for u in range(max(r, 0), 4):
                            S.mm(acc1(accs[c][e], u), pts[e][:, u * 128:(u + 1) * 128],
                                 vtm[:, kt, 0 * 2 + g, :], start=(kt == 0 and u == 0), stop=True, skip_group_check=True)

                def make_mask(kt):
                    r = kt - 4 * Q
                    c0 = 128 * r if r > 0 else 0
                    mp = next_st()
                    S.mm(mp[:, c0:512], CBs("esel", slice(0, 128), slice(kt * 128, (kt + 1) * 128)),
                         selT[:, g, Q * 512 + c0:(Q + 1) * 512], start=True, stop=True)
                    ms = Msb[msb_cnt[0] % 2]
                    msb_cnt[0] += 1
                    S.act(ms[:, c0:512], mp[:, c0:512], AF.Copy)
                    if r >= 0:
                        S.tt(ms[:, c0:c0 + 128], ms[:, c0:c0 + 128], CBs("tri"), ALU.mult, eng="pool")
                    return ms

                ms_next = make_mask(0)
                for kt in range(nk):
                    r = kt - 4 * Q
                    c0 = 128 * r if r > 0 else 0
                    ms = ms_next
                    if kt + 1 < nk:
                        ms_next = make_mask(kt + 1)
                    for c in cs:
                        hs = (2 * c, 2 * c + 1)
                        stp = [next_st(), next_st()]
                        for e in range(2):
                            lo, hi = HALF[e]
                            S.mm(stp[e][:, c0:512], kslc[lo:hi, g, kt * 128:(kt + 1) * 128],
                                 qaT[lo:hi, c, Q * 512 + c0:(Q + 1) * 512], start=True, stop=True)
                        pts = [next_pt(), next_pt()]
                        for e in range(2):
                            h = hs[e]
                            bcol = h * 19 + (kt - 4 * Q + 15)
                            S.act(pts[e][:, c0:512], stp[e][:, c0:512], AF.Exp,
                                  bias=CFs("bias_slc", slice(0, 128), slice(bcol, bcol + 1)))
                            S.tt(pts[e][:, c0:512], pts[e][:, c0:512], ms[:, c0:512], ALU.mult)
                        pend.append((kt, c, pts))
                        if len(pend) > 1:
                            pv(*pend.pop(0))
                while pend:
                    pv(*pend.pop(0))
                for c in cs:
                    for e in range(2):
                        evac_nsa(accs[c][e], Q, 2 * c + e, 1, False)

        band_cnt = [0]

        def do_band(Q, which):
            nwin = 3 if which == "win" else 2
            if which == "win":
                qT_, kT_, vbase, band = qaT, kwin, 1 * 2, "band_win"
            else:
                qT_, kT_, vbase, band = qbT, kb, 2 * 2, "band_swa"
            bc0, _ = CB[band]
            pend = []

            def finish(step):
                c, u, js, pts, accs, i = step
                g = c // 2
                hs = (2 * c, 2 * c + 1)
                for e in range(2):
                    for j in js:
                        mi = j - (i - nwin + 1)
                        S.mm(acc1(accs[e], u), pts[e][:, mi * 128:(mi + 1) * 128],
                             vtm[:, j, vbase + g, :], start=(u == 0 and j == js[0]), stop=True, skip_group_check=True)
                if u == 3:
                    for e in range(2):
                        h = hs[e]
                        if which == "win":
                            evac_nsa(accs[e], Q, h, 2, False)
                        else:
                            z = accv(accs[e], 0, 4, 64, 65)
                            S.ts(ev_rz.with_ap(lambda a: a.unsqueeze(2)), z, sinkt[:, h:h + 1], None, ALU.add)
                            S.recip(ev_rz, ev_rz)
                            cb_ = ev_rz.with_ap(lambda a: a.unsqueeze(2).broadcast_to([128, 4, 64]))
                            S.tt(ob[:, 4 * Q:4 * Q + 4, h, :], accv(accs[e], 0, 4, 0, 64), cb_, ALU.mult)

            for c in range(4):
                g = c // 2
                hs = (2 * c, 2 * c + 1)
                ab = 4 + 2 * (band_cnt[0] % 2)
                band_cnt[0] += 1
                accs = [PS[ab], PS[ab + 1]]
                for u in range(4):
                    i = 4 * Q + u
                    js = [j for j in range(i - nwin + 1, i + 1) if j >= 0]
                    stp = [next_st(), next_st()]
                    for j in js:
                        mi = j - (i - nwin + 1)
                        for e in range(2):
                            lo, hi = HALF[e]
                            S.mm(stp[e][:, mi * 128:(mi + 1) * 128], kT_[lo:hi, g, j * 128:(j + 1) * 128],
                                 qT_[lo:hi, c, i * 128:(i + 1) * 128], start=(j == js[0]), stop=True, skip_group_check=True)
                    mi0 = js[0] - (i - nwin + 1)
                    pts = [next_pt(), next_pt()]
                    for e in range(2):
                        h = hs[e]
                        hcol = h if which == "win" else 8 + h
                        S.act(pts[e][:, mi0 * 128:nwin * 128], stp[e][:, mi0 * 128:nwin * 128], AF.Exp,
                              bias=CFs("bias_band", slice(0, 128), slice(hcol, hcol + 1)))
                        boff = bc0 + (h * nwin + mi0) * 128
                        S.tt(pts[e][:, mi0 * 128:nwin * 128], pts[e][:, mi0 * 128:nwin * 128],
                             constb[:, boff:boff + (nwin - mi0) * 128], ALU.mult, eng=("pool" if (which == "win" and e == 0) else "dve"))
                    pend.append((c, u, js, pts, accs, i))
                    if len(pend) > 1:
                        finish(pend.pop(0))
            while pend:
                finish(pend.pop(0))

        for Q in range(4):
            do_cmp(Q)
            do_select(Q)
            do_slc(Q)
            do_band(Q, "win")
            do_band(Q, "swa")
        tap("selT", selT[0:32, :, :], [32, 2, SEQ], BF16)
        tap("selT2", selT[64:96, :, :], [32, 2, SEQ], BF16)
        tap("oa", oa[:, :, :, :], [128, NT, 8, 64], F32)
        tap("ob", ob[:, :, :, :], [128, NT, 8, 64], BF16)
        if stop == "attn":
            S.emit()
            return nc

        oaT = Buf(qaT.h, qaT.shape, BF16, "oaT", base_byte=qaT.base_byte, alias=ARENA)
        obT = Buf(qbT.h, qbT.shape, BF16, "obT", base_byte=qbT.base_byte, alias=ARENA)
        RM = Region(R_K.base, R_K.size + R_V.size + R_SEL.size + R_PT.size)
        RM.take("wgs01_reserved", [2, 8, 256], BF16)
        wa = RM.take("wa", [4, DM], BF16)
        wbm = RM.take("wbm", [4, DM], BF16)
        wo = RM.take("wo", [8, DM], BF16)
        wgs[2] = RM.take("wgs2", [8, 256], BF16)
        wgs[3] = RM.take("wgs3", [8, 256], BF16)
        oab = [RM.take("oab%d" % i, [512], BF16) for i in range(2)]
        sg = [RM.take("sg%d" % i, [512], F32) for i in range(2)]
        tA = RM.take("tA", [512], F32)
        tB = RM.take("tB", [512], F32)
        mixT = Buf(arena_h[:, R_OA.base // 2:(R_OA.base + 32768) // 2].rearrange("p (a b) -> p a b", a=8), [128, 8, SEQ], BF16,
                   "mixT", base_byte=R_OA.base, alias=ARENA)
        RX = Region(R_OB.base, R_OB.size)
        xr = [RX.take("xr%d" % i, [DM], F32) for i in range(2)]

        sqjunk = Buf(arena_h[:, sg[0].base_byte // 2:sg[0].base_byte // 2 + 1024], [128, DM], BF16, "sqjunk",
                     base_byte=sg[0].base_byte, alias=ARENA)
        S.dma(wa[:, :, :], wa_d.rearrange("(kc p) n -> p kc n", p=128), eng="pool")
        S.dma(wbm[:, :, :], wb_d.rearrange("(kc p) n -> p kc n", p=128), eng="pool")
        S.dma(wo[:, :, :], wo_d.rearrange("(kc p) n -> p kc n", p=128), eng="pool")
        obv = Buf(ob.h.rearrange("p t h d -> p t (h d)"), [128, NT, 512], BF16, "obv", base_byte=ob.base_byte, alias=ARENA)
        for t in range(NT):
            o_ = oab[t % 2]
            S.copy(o_[:, :], r3(oa[:, t, :, :], "p h d -> p (h d)"), eng="act")
            pb = PSB[t % 2]
            for c in range(4):
                S.transpose(pb[:, c * 128:(c + 1) * 128], o_[:, c * 128:(c + 1) * 128], identb)
            for c in range(4):
                S.transpose(pb[:, 512 + c * 128:512 + (c + 1) * 128], obv[:, t, c * 128:(c + 1) * 128], identb)
            S.copy(oaT[:, :, t * 128:(t + 1) * 128], r3(pb[:, 0:512], "p (c n) -> p c n", c=4), eng="dve")
            S.copy(obT[:, :, t * 128:(t + 1) * 128], r3(pb[:, 512:1024], "p (c n) -> p c n", c=4), eng="dve")
        n = 0
        for fo in range(8):
            if fo % 2 == 0:
                if fo // 2 + 1 < 4:
                    load_gate_piece(fo // 2 + 1)
            for tb in range(4):
                ts_ = slice(tb * 512, (tb + 1) * 512)
                res = []
                for ab, (wproj, oT, tdst) in enumerate(((wa, oaT, tA), (wbm, obT, tB))):
                    pg = PS[2 + n % 2]
                    pa = PS[4 + n % 2]
                    n += 1
                    for kc in range(8):
                        S.mm(pg[:, :], gpieces[(fo // 2, ab)][:, kc, (fo % 2) * 128:(fo % 2 + 1) * 128], hT[:, kc, ts_],
                             start=(kc == 0), stop=(kc == 7))
                    for ki in range(4):
                        S.mm(pa[:, :], wproj[:, ki, fo * 128:(fo + 1) * 128], oT[:, ki, ts_], start=(ki == 0), stop=(ki == 3))
                    s_ = sg[ab]
                    S.act(s_[:, :], pg[:, :], AF.Sigmoid)
                    S.tt(tdst[:, :], pa[:, :], s_[:, :], ALU.mult)
                S.tt(mixT[:, fo, ts_], tA[:, :], tB[:, :], ALU.add, eng="pool")
        x1 = Buf(arena_h[:, R_HT.base // 2:(R_HT.base + 65536) // 2].bitcast(F32).rearrange("p (t d) -> p t d", t=NT),
                 [128, NT, DM], F32, "x1", base_byte=R_HT.base, alias=ARENA)
        for t in range(NT):
            xs = xr[t % 2]
            S.dma(xs[:, :], x_d[t * 128:(t + 1) * 128, :])
            for dh in range(2):
                p = PS[6 + dh]
                for fo in range(8):
                    S.mm(p[:, :], mixT[:, fo, t * 128:(t + 1) * 128], wo[:, fo, dh * 512:(dh + 1) * 512],
                         start=(fo == 0), stop=(fo == 7))
                S.tt(x1[:, t, dh * 512:(dh + 1) * 512], p[:, :], xs[:, dh * 512:(dh + 1) * 512], ALU.add)
            S.act(sqjunk[:, :], x1[:, t, :], AF.Square, accum=ssq[:, t:t + 1])
        tap("x1", x1[:, :, :], [128, NT, DM], F32)
        if stop == "merge":
            S.emit()
            return nc

        h2T = Buf(arena_h[:, R_OA.base // 2:(R_OA.base + 32768) // 2].rearrange("p (a b) -> p a b", a=8), [128, 8, SEQ], BF16,
                  "h2T", base_byte=R_OA.base, alias=ARENA)
        RE = Region(R_K.base, R_K.size + R_V.size + R_SEL.size + R_PT.size)
        Wt = RE.take("Wt", [NT, 32], F32)
        wgu = [RE.take("wgu%d" % i, [2, 8, 256], BF16) for i in range(2)]
        wdn = [RE.take("wdn%d" % i, [2, DM], BF16) for i in range(3)]
        AT = [RE.take("AT%d" % i, [2, SEQ], BF16) for i in range(2)]
        sGs = [RE.take("sG%d" % i, [512], BF16) for i in range(2)]
        RR = Region(R_OB.base, R_OB.size)
        gbc2 = RR.take("gbc2", [DM], F32)
        h2f_a = RR.take("h2f", [DM], F32)
        h2b_a = RR.take("h2b", [DM], BF16)
        h2T32_a = RR.take("h2T32", [8, 128], F32)
        wr32 = RR.take("wr32", [8, 36], F32)
        RR2 = Region(AT[0].base_byte, 16384)
        h2f_b = RR2.take("h2f_b", [DM], F32)
        h2b_b = RR2.take("h2b_b", [DM], BF16)
        h2T32_b = RR2.take("h2T32_b", [8, 128], F32)
        h2b = h2b_a
        lgTs = [RR2.take("lgT%d" % i, [128], F32) for i in range(2)]
        RS = RE
        Lg = RS.take("Lg", [NT, 36], F32)
        bias36 = RS.take("bias36", [36], F32)
        ss2 = RS.take("ss2", [NT], F32)
        rstd2 = RS.take("rstd2", [NT], F32)
        rt = [RS.take("rt%d" % i, [NT], F32) for i in range(6)]
        gm4 = RS.take("gm4", [NT, 4], F32)
        ge4 = RS.take("ge4", [NT, 4], F32)
        m1 = RS.take("m1", [NT, 32], F32)
        m2 = RS.take("m2", [NT, 32], F32)
        em = RS.take("em", [NT, 32], F32)

        S.dma(gbc2[:, :], gffn_d.partition_broadcast(128))
        S.dma(wr32[:, :, 0:4], wgrp_d.rearrange("(kc p) n -> p kc n", p=128))
        S.dma(wr32[:, :, 4:36], wexp_d.rearrange("(kc p) n -> p kc n", p=128))
        S.dma(bias36[:, 0:4], bgrp_d.partition_broadcast(128))
        S.dma(bias36[:, 4:36], bexp_d.partition_broadcast(128))
        S.ts(rstd2[:, :], ssq[:, :], 1.0 / DM, 1e-6, ALU.mult, ALU.add)
        S.act(rstd2[:, :], rstd2[:, :], AF.Sqrt)
        S.recip(rstd2[:, :], rstd2[:, :])
        for t in range(NT):
            h2f, h2b, h2T32 = (h2f_a, h2b_a, h2T32_a) if t % 2 == 0 else (h2f_b, h2b_b, h2T32_b)
            S.stt(h2f[:, :], x1[:, t, :], rstd2[:, t:t + 1], gbc2[:, :], ALU.mult, ALU.mult)
            for half in range(2):
                pf = PS[(2 if t % 2 == 0 else 6) + half]
                for c in range(4):
                    cc = half * 4 + c
                    S.transpose(pf[:, c * 128:(c + 1) * 128], h2f[:, cc * 128:(cc + 1) * 128], identf)
                S.copy(h2T32[:, half * 4:(half + 1) * 4, :], r3(pf[:, :], "p (c n) -> p c n", c=4), eng="act")
                S.copy(h2T[:, half * 4:(half + 1) * 4, t * 128:(t + 1) * 128], h2T32[:, half * 4:(half + 1) * 4, :], eng="dve")
            pl = PS[4 + t % 2]
            for c in range(8):
                S.mm(pl[0:36, 0:128], wr32[:, c, :], h2T32[:, c, :], start=(c == 0), stop=(c == 7))
            lgT = lgTs[t % 2]
            S.copy(lgT[0:36, :], pl[0:36, 0:128], eng="act")
            S.transpose(pl[:, 256:292], lgT[0:36, :], CFs("identf", slice(0, 36), slice(0, 36)))
            S.tt(Lg[:, t, :], pl[:, 256:292], bias36[:, :], ALU.add)
        gl = Lg[:, :, 0:4]
        el = Lg[:, :, 4:36]
        gmax, gsum, top1, top2, w1_, a1 = rt
        S.reduce(gmax[:, :], gl, ALU.max)
        gmb = gmax[:, :].with_ap(lambda a: a.unsqueeze(2).broadcast_to([128, NT, 4]))
        S.tt(gm4[:, :, :], gl, gmb, ALU.is_equal)
        S.tt(ge4[:, :, :], gl, gmb, ALU.subtract)
        S.act(ge4[:, :, :], ge4[:, :, :], AF.Exp)
        S.reduce(gsum[:, :], ge4[:, :, :], ALU.add)
        S.recip(gsum[:, :], gsum[:, :])
        S.ts(gm4[:, :, :], gm4[:, :, :], -1.0, 1e30, ALU.add, ALU.mult)
        pen = gm4[:, :, :].with_ap(lambda a: a.unsqueeze(3).broadcast_to([128, NT, 4, 8]))
        S.tt(r3(em[:, :, :], "p t (g e) -> p t g e", g=4), r3(el, "p t (g e) -> p t g e", g=4), pen, ALU.add)
        S.reduce(top1[:, :], em[:, :, :], ALU.max)
        S.tt(m1[:, :, :], em[:, :, :], top1[:, :].with_ap(lambda a: a.unsqueeze(2).broadcast_to([128, NT, 32])), ALU.is_equal)
        S.stt(em[:, :, :], m1[:, :, :], -1e30, em[:, :, :], ALU.mult, ALU.add)
        S.reduce(top2[:, :], em[:, :, :], ALU.max)
        S.tt(m2[:, :, :], em[:, :, :], top2[:, :].with_ap(lambda a: a.unsqueeze(2).broadcast_to([128, NT, 32])), ALU.is_equal)
        S.tt(w1_[:, :], top2[:, :], top1[:, :], ALU.subtract)
        S.act(w1_[:, :], w1_[:, :], AF.Exp)
        S.ts(w1_[:, :], w1_[:, :], 1.0, None, ALU.add)
        S.recip(w1_[:, :], w1_[:, :])
        S.tt(a1[:, :], gsum[:, :], w1_[:, :], ALU.mult)
        S.tt(top2[:, :], gsum[:, :], a1[:, :], ALU.subtract)
        S.tt(m1[:, :, :], m1[:, :, :], a1[:, :].with_ap(lambda a: a.unsqueeze(2).broadcast_to([128, NT, 32])), ALU.mult)
        S.tt(m2[:, :, :], m2[:, :, :], top2[:, :].with_ap(lambda a: a.unsqueeze(2).broadcast_to([128, NT, 32])), ALU.mult)
        S.tt(Wt[:, :, :], m1[:, :, :], m2[:, :, :], ALU.add)
        tap("Wt", Wt[:, :, :], [128, NT, 32], F32)
        tap("Lg", Lg[:, :, :], [128, NT, 36], F32)
        if stop == "route":
            S.emit()
            return nc

        wg_v = wg_d.rearrange("e (kc p) f -> e p kc f", p=128)
        wu_v = wu_d.rearrange("e (kc p) f -> e p kc f", p=128)
        wd_v = wd_d.rearrange("e (fc p) n -> e p fc n", p=128)

        def load_expert(e):
            S.dma(wgu[e % 2][:, 0, :, :], wg_v[e], eng="pool")
            S.dma(wgu[e % 2][:, 1, :, :], wu_v[e], eng="pool")
            S.dma(wdn[e % 3][:, :, :], wd_v[e], eng="pool")

        NE = 32
        load_expert(0)
        n = 0
        ny = 0

        def down_units(e):
            A_ = AT[e % 2]
            wd_ = wdn[e % 3]
            units = []
            for t in range(NT):
                for dh in range(2):
                    units.append((e, A_, wd_, t, dh))
            return units

        def emit_down(unit):
            nonlocal ny
            e, A_, wd_, t, dh = unit
            py = PS[4 + ny % 4]
            ny += 1
            for fc in range(2):
                S.mm(py[:, :], A_[:, fc, t * 128:(t + 1) * 128], wd_[:, fc, dh * 512:(dh + 1) * 512],
                     start=(fc == 0), stop=(fc == 1))
            xv = x1[:, t, dh * 512:(dh + 1) * 512]
            S.stt(xv, py[:, :], Wt[:, t, e:e + 1], xv, ALU.mult, ALU.add)

        pending = []
        for e in range(NE):
            if e + 1 < NE:
                load_expert(e + 1)
            w = wgu[e % 2]
            A = AT[e % 2]
            for tb in range(4):
                ts_ = slice(tb * 512, (tb + 1) * 512)
                for fc in range(2):
                    pg = PS[0 + n % 2]
                    pu = PS[2 + n % 2]
                    n += 1
                    for kc in range(8):
                        S.mm(pg[:, :], w[:, 0, kc, fc * 128:(fc + 1) * 128], h2T[:, kc, ts_], start=(kc == 0), stop=(kc == 7))
                        if kc in (3, 7) and pending:
                            emit_down(pending.pop(0))
                    s_ = sGs[n % 2]
                    S.act(s_[:, :], pg[:, :], AF.Silu)
                    for kc in range(8):
                        S.mm(pu[:, :], w[:, 1, kc, fc * 128:(fc + 1) * 128], h2T[:, kc, ts_], start=(kc == 0), stop=(kc == 7))
                        if kc in (3, 7) and pending:
                            emit_down(pending.pop(0))
                    S.tt(A[:, fc, ts_], pu[:, :], s_[:, :], ALU.mult)
            while pending:
                emit_down(pending.pop(0))
            pending = down_units(e)
        RF = Region(R_OB.base, R_OB.size)
        gbc3 = RF.take("gbc3", [DM], F32)
        ot = [RF.take("ot%d" % i, [DM], F32) for i in range(2)]
        jk = RF.take("jk", [DM], BF16)
        S.dma(gbc3[:, :], gfin_d.partition_broadcast(128))
        while pending:
            unit = pending.pop(0)
            emit_down(unit)
            t, dh = unit[3], unit[4]
            if dh == 1:
                S.act(jk[:, :], x1[:, t, :], AF.Square, accum=ss2[:, t:t + 1])
                if t % 4 == 3:
                    t4 = t - 3
                    S.ts(rstd2[:, t4:t4 + 4], ss2[:, t4:t4 + 4], 1.0 / DM, 1e-6, ALU.mult, ALU.add)
                    S.act(rstd2[:, t4:t4 + 4], rstd2[:, t4:t4 + 4], AF.Sqrt)
                    S.recip(rstd2[:, t4:t4 + 4], rstd2[:, t4:t4 + 4])
                    for t_ in range(t4, t4 + 4):
                        o_ = ot[t_ % 2]
                        S.stt(o_[:, :], x1[:, t_, :], rstd2[:, t_:t_ + 1], gbc3[:, :], ALU.mult, ALU.mult)
                        S.dma(out_d[t_ * 128:(t_ + 1) * 128, :], o_[:, :], is_out=True)
        S.emit()
    return nc


_W_NAMES = ["norm_mix_g", "w_in", "cmp_pos_k", "cmp_w1_k", "cmp_w2_k", "cmp_pos_v", "cmp_w1_v", "cmp_w2_v", "sinks",
            "w_a", "w_b", "w_o", "norm_ffn_g", "w_group", "b_group", "w_expert", "b_expert",
            "w_gate_e", "w_up_e", "w_down_e"]


def make_in_maps(inputs, n_cores=8):
    cb, cf = make_consts()
    shared = {"constb": cb, "constf": cf}
    for k in _W_NAMES:
        a = np.ascontiguousarray(np.asarray(inputs[k], dtype=np.float32))
        shared[k] = a.reshape(a.shape[1:]) if a.shape[0] == 1 and a.ndim >= 2 else a
    for k in ("norm_mix_g", "sinks", "norm_ffn_g", "b_group", "b_expert"):
        shared[k] = shared[k].reshape(1, -1)
    shared["norm_final_g"] = np.ascontiguousarray(np.asarray(inputs["norm_final_g"], np.float32)).reshape(1, -1)
    x = np.asarray(inputs["x"], dtype=np.float32)
    return [dict(shared, x=np.ascontiguousarray(x[c])) for c in range(n_cores)]


_NC_CACHE = {}


def kernel(**inputs):
    if "nc" not in _NC_CACHE:
        _NC_CACHE["nc"] = build_program()
    nc = _NC_CACHE["nc"]
    in_maps = make_in_maps(inputs, 8)
    res = run_bass_kernel_spmd(nc, in_maps, core_ids=list(range(8)))
    out = np.stack([np.asarray(r["out"], dtype=np.float32) for r in res.results], axis=0)
    return out
```
